# Optimizing a Trainium2 kernel written in Bass

```python
import jax, jax.numpy as jnp
from jax import lax
import numpy as np

D_MODEL = 2048
BATCH = 4
SEQ = 4096
DEPTH = 4

MLA_HEADS = 16
MLA_Q_LORA = 512
MLA_KV_LORA = 512
MLA_NOPE_DIM = 128
MLA_ROPE_DIM = 64
MLA_V_DIM = 128
ROPE_THETA = 10000.0
MOBA_HEADS = 16
MOBA_HEAD_DIM = 128
MOBA_BLOCK = 256
MOBA_TOP_BLOCKS = 3
MOBA_QUERY_ROWS = 128
N_EXPERTS = 32
TOP_K = 4
EXPERT_FF = 1024
SWIGLU_LIMIT = 7.0
SWIGLU_ALPHA = 1.702
MOE_ROWS = 128
Q_BLOCK = 128
N_MLA_LAYERS = (DEPTH + 1) // 2
N_MOBA_LAYERS = DEPTH // 2
DEEPNORM_ALPHA = (2.0 * DEPTH) ** 0.25
DEEPNORM_BETA = (8.0 * DEPTH) ** -0.25
NEG_INF = -1e30
LN_EPS = 1e-5
RMS_EPS = 1e-6

kernel_name = "hybrid_mla_moba_moe_deepnorm"


def layer_norm(x, g, b):
    xf = x.astype(jnp.float32)
    mu = jnp.mean(xf, axis=-1, keepdims=True)
    var = jnp.mean(jnp.square(xf - mu), axis=-1, keepdims=True)
    return ((xf - mu) * lax.rsqrt(var + LN_EPS) * g + b).astype(x.dtype)


def rms_norm(x, g):
    xf = x.astype(jnp.float32)
    return (xf * lax.rsqrt(jnp.mean(jnp.square(xf), axis=-1, keepdims=True) + RMS_EPS) * g).astype(x.dtype)


def rope_tables(positions):
    inv_freq = ROPE_THETA ** (-jnp.arange(0, MLA_ROPE_DIM, 2, dtype=jnp.float32) / MLA_ROPE_DIM)
    ang = positions.astype(jnp.float32)[..., None] * inv_freq
    return jnp.cos(ang), jnp.sin(ang)


def apply_rope(x, cos, sin):
    x1, x2 = jnp.split(x.astype(jnp.float32), 2, axis=-1)
    return jnp.concatenate([x1 * cos - x2 * sin, x2 * cos + x1 * sin], axis=-1).astype(x.dtype)


def alibi_slopes(n_heads):
    return 2.0 ** (-8.0 * jnp.arange(1, n_heads + 1, dtype=jnp.float32) / n_heads)


def attend(s, v, eq):
    m = jnp.max(s, axis=-1, keepdims=True)
    p = jnp.exp(s - m)
    l = jnp.sum(p, axis=-1)
    o = jnp.einsum(eq, p.astype(v.dtype), v).astype(jnp.float32) / l[..., None]
    return o, m[..., 0] + jnp.log(l)


def group_rows(group_ids, num_groups, rows):
    n = group_ids.shape[0]
    n_blocks = -(-n // rows) + num_groups
    order = jnp.argsort(group_ids)
    sorted_g = group_ids[order]
    counts = jnp.zeros((num_groups,), jnp.int32).at[group_ids].add(1)
    padded = (counts + rows - 1) // rows * rows
    start = jnp.cumsum(counts) - counts
    pstart = jnp.cumsum(padded) - padded
    dest = pstart[sorted_g] + (jnp.arange(n, dtype=jnp.int32) - start[sorted_g])
    row_src = jnp.zeros((n_blocks * rows,), jnp.int32).at[dest].set(order.astype(jnp.int32))
    row_of = jnp.zeros((n,), jnp.int32).at[order].set(dest)
    block_starts = jnp.arange(n_blocks, dtype=jnp.int32) * rows
    block_group = jnp.searchsorted(jnp.cumsum(padded), block_starts, side='right')
    block_group = jnp.minimum(block_group, num_groups - 1).astype(jnp.int32)
    return row_src, block_group, row_of


def mla_attention(x, positions, w_in, g_q, g_kv, w_qb, w_kvb, w_o):
    B, S, _ = x.shape
    H = MLA_HEADS
    c = x @ w_in
    c_q, c_kv, k_rope = jnp.split(c, [MLA_Q_LORA, MLA_Q_LORA + MLA_KV_LORA], axis=-1)
    q = (rms_norm(c_q, g_q) @ w_qb).reshape(B, S, H, MLA_NOPE_DIM + MLA_ROPE_DIM)
    kv = (rms_norm(c_kv, g_kv) @ w_kvb).reshape(B, S, H, MLA_NOPE_DIM + MLA_V_DIM)
    q_nope, q_rope = jnp.split(q, [MLA_NOPE_DIM], axis=-1)
    k_nope, v = jnp.split(kv, [MLA_NOPE_DIM], axis=-1)
    cos, sin = rope_tables(positions)
    q_rope = apply_rope(q_rope, cos[:, :, None], sin[:, :, None])
    k_rope = apply_rope(k_rope, cos, sin)
    scale = (MLA_NOPE_DIM + MLA_ROPE_DIM) ** -0.5
    nqb = S // Q_BLOCK
    qn_b = q_nope.reshape(B, nqb, Q_BLOCK, H, MLA_NOPE_DIM).swapaxes(0, 1)
    qr_b = q_rope.reshape(B, nqb, Q_BLOCK, H, MLA_ROPE_DIM).swapaxes(0, 1)
    key_idx = jnp.arange(S)

    def q_block(args):
        qn, qr, i = args
        s = (jnp.einsum('bqhd,bkhd->bhqk', qn, k_nope)
             + jnp.einsum('bqhr,bkr->bhqk', qr, k_rope)).astype(jnp.float32) * scale
        q_idx = i * Q_BLOCK + jnp.arange(Q_BLOCK)
        s = jnp.where(key_idx[None, :] <= q_idx[:, None], s, NEG_INF)
        p = jax.nn.softmax(s, axis=-1)
        return jnp.einsum('bhqk,bkhd->bqhd', p.astype(v.dtype), v)

    o = lax.map(q_block, (qn_b, qr_b, jnp.arange(nqb)))
    o = o.swapaxes(0, 1).reshape(B, S, H * MLA_V_DIM)
    return o @ w_o


def moba_attention(x, positions, w_qkv, w_o):
    B, S, _ = x.shape
    H, Dh, BS = MOBA_HEADS, MOBA_HEAD_DIM, MOBA_BLOCK
    nb = -(-S // BS)
    S_pad = nb * BS
    k_sel = min(MOBA_TOP_BLOCKS, nb)
    scale = Dh ** -0.5
    qkv = (x @ w_qkv).reshape(B, S, 3, H, Dh)
    qkv = jnp.pad(qkv, ((0, 0), (0, S_pad - S), (0, 0), (0, 0), (0, 0)))
    q = jnp.moveaxis(qkv[:, :, 0], 1, 2)
    k = jnp.moveaxis(qkv[:, :, 1], 1, 2)
    v = jnp.moveaxis(qkv[:, :, 2], 1, 2)
    pos = jnp.pad(positions, ((0, 0), (0, S_pad - S)), mode='edge').astype(jnp.float32)
    slopes = alibi_slopes(H)
    qb = q.reshape(B, H, nb, BS, Dh)
    kb = k.reshape(B, H, nb, BS, Dh)
    vb = v.reshape(B, H, nb, BS, Dh)
    posb = pos.reshape(B, nb, BS)

    kmean = jnp.mean(kb.astype(jnp.float32), axis=3)
    gate = jnp.einsum('bhsd,bhmd->bhsm', q.astype(jnp.float32), kmean)
    q_blk = jnp.arange(S_pad) // BS
    gate = jnp.where(jnp.arange(nb)[None, :] < q_blk[:, None], gate, NEG_INF)
    _, sel = lax.top_k(gate, k_sel)
    sel_valid = sel < q_blk[:, None]

    s_own = jnp.einsum('bhnqd,bhnkd->bhnqk', qb, kb).astype(jnp.float32) * scale
    dist = posb[:, :, :, None] - posb[:, :, None, :]
    s_own = s_own - slopes[None, :, None, None, None] * dist[:, None]
    s_own = jnp.where(jnp.tril(jnp.ones((BS, BS), bool)), s_own, NEG_INF)
    o_own, lse_own = attend(s_own, vb, 'bhnqk,bhnkd->bhnqd')
    o_own = o_own.reshape(B, H, S_pad, Dh)
    lse_own = lse_own.reshape(B, H, S_pad, 1)

    G = B * H * nb
    g_ids = ((jnp.arange(B)[:, None, None, None] * H + jnp.arange(H)[None, :, None, None]) * nb
             + sel).reshape(-1).astype(jnp.int32)
    row_src, block_g, row_of = group_rows(g_ids, G, MOBA_QUERY_ROWS)
    n_blk = block_g.shape[0]
    q_src = row_src // k_sel
    q_rows = q.reshape(-1, Dh)[q_src].reshape(n_blk, MOBA_QUERY_ROWS, Dh)
    pq_rows = jnp.broadcast_to(pos[:, None, :], (B, H, S_pad)).reshape(-1)[q_src]
    pq_rows = pq_rows.reshape(n_blk, MOBA_QUERY_ROWS)
    k_flat = kb.reshape(G, BS, Dh)
    v_flat = vb.reshape(G, BS, Dh)
    pk_flat = jnp.broadcast_to(posb[:, None], (B, H, nb, BS)).reshape(G, BS)
    slope_flat = jnp.broadcast_to(slopes[None, :, None], (B, H, nb)).reshape(G)

    def sel_block(args):
        qr, pq, g = args
        s = (qr @ k_flat[g].T).astype(jnp.float32) * scale
        s = s - slope_flat[g] * (pq[:, None] - pk_flat[g][None, :])
        return attend(s, v_flat[g], 'qk,kd->qd')

    o_rows, lse_rows = lax.map(sel_block, (q_rows, pq_rows, block_g))
    o_sel = o_rows.reshape(-1, Dh)[row_of].reshape(B, H, S_pad, k_sel, Dh)
    lse_sel = lse_rows.reshape(-1)[row_of].reshape(B, H, S_pad, k_sel)
    lse_sel = jnp.where(sel_valid, lse_sel, NEG_INF)

    w = jax.nn.softmax(jnp.concatenate([lse_own, lse_sel], axis=-1), axis=-1)
    o = w[..., :1] * o_own + jnp.einsum('bhsk,bhskd->bhsd', w[..., 1:], o_sel)
    o = o[:, :, :S].astype(x.dtype)
    return jnp.moveaxis(o, 1, 2).reshape(B, S, H * Dh) @ w_o


def moe_ffn(x2, w_router, b_router, w_gu, b_gu, w_down, b_down):
    T, D = x2.shape
    logits = (x2 @ w_router + b_router).astype(jnp.float32)
    top_val, top_idx = lax.top_k(logits, TOP_K)
    gates = jax.nn.softmax(top_val, axis=-1).astype(x2.dtype)
    row_src, block_e, row_of = group_rows(top_idx.reshape(-1).astype(jnp.int32), N_EXPERTS, MOE_ROWS)
    n_blk = block_e.shape[0]
    xs = x2[row_src // TOP_K].reshape(n_blk, MOE_ROWS, D)

    def expert_block(args):
        xb, e = args
        h = xb @ w_gu[e] + b_gu[e]
        x_glu, x_lin = jnp.split(h, 2, axis=-1)
        x_glu = jnp.minimum(x_glu, SWIGLU_LIMIT)
        x_lin = jnp.clip(x_lin, -SWIGLU_LIMIT, SWIGLU_LIMIT)
        act = x_glu * jax.nn.sigmoid(SWIGLU_ALPHA * x_glu) * (x_lin + 1.0)
        return act @ w_down[e] + b_down[e]

    ys = lax.map(expert_block, (xs, block_e)).reshape(n_blk * MOE_ROWS, D)
    y_assign = ys[row_of].reshape(T, TOP_K, D)
    return jnp.einsum('tk,tkd->td', gates, y_assign)


def setup_inputs(seed: int = 0) -> dict:
    key = jax.random.key(seed)
    ks = jax.random.split(key, 20)
    f32 = jnp.float32
    nrm = lambda k, shape, s: jax.random.normal(k, shape, f32) * s
    LA, LB, H = N_MLA_LAYERS, N_MOBA_LAYERS, MLA_HEADS
    beta = DEEPNORM_BETA
    x = jax.random.normal(ks[0], (BATCH, SEQ, D_MODEL), f32)
    positions = (jax.random.randint(ks[1], (BATCH, 1), 0, 1024, dtype=jnp.int32)
                 + jnp.arange(SEQ, dtype=jnp.int32)[None, :])
    mla_w_in = nrm(ks[2], (LA, D_MODEL, MLA_Q_LORA + MLA_KV_LORA + MLA_ROPE_DIM), D_MODEL ** -0.5)
    mla_g_q = 1.0 + nrm(ks[3], (LA, MLA_Q_LORA), 0.02)
    mla_g_kv = 1.0 + nrm(ks[4], (LA, MLA_KV_LORA), 0.02)
    mla_w_qb = nrm(ks[5], (LA, MLA_Q_LORA, H * (MLA_NOPE_DIM + MLA_ROPE_DIM)), MLA_Q_LORA ** -0.5)
    kv_col_scale = jnp.concatenate([jnp.ones((MLA_NOPE_DIM,), f32), jnp.full((MLA_V_DIM,), beta, f32)])
    mla_w_kvb = (nrm(ks[6], (LA, MLA_KV_LORA, H, MLA_NOPE_DIM + MLA_V_DIM), MLA_KV_LORA ** -0.5)
                 * kv_col_scale).reshape(LA, MLA_KV_LORA, H * (MLA_NOPE_DIM + MLA_V_DIM))
    mla_w_o = nrm(ks[7], (LA, H * MLA_V_DIM, D_MODEL), (H * MLA_V_DIM) ** -0.5 * beta)
    qkv_scale = jnp.array([1.0, 1.0, beta], f32)[:, None]
    moba_w_qkv = (nrm(ks[8], (LB, D_MODEL, 3, MOBA_HEADS * MOBA_HEAD_DIM), D_MODEL ** -0.5)
                  * qkv_scale).reshape(LB, D_MODEL, 3 * MOBA_HEADS * MOBA_HEAD_DIM)
    moba_w_o = nrm(ks[9], (LB, MOBA_HEADS * MOBA_HEAD_DIM, D_MODEL), (MOBA_HEADS * MOBA_HEAD_DIM) ** -0.5 * beta)
    ln1_g = 1.0 + nrm(ks[10], (DEPTH, D_MODEL), 0.02)
    ln1_b = nrm(ks[11], (DEPTH, D_MODEL), 0.02)
    ln2_g = 1.0 + nrm(ks[12], (DEPTH, D_MODEL), 0.02)
    ln2_b = nrm(ks[13], (DEPTH, D_MODEL), 0.02)
    moe_w_router = nrm(ks[14], (DEPTH, D_MODEL, N_EXPERTS), D_MODEL ** -0.5)
    moe_b_router = nrm(ks[15], (DEPTH, N_EXPERTS), 0.01)
    moe_w_gu = nrm(ks[16], (DEPTH, N_EXPERTS, D_MODEL, 2 * EXPERT_FF), D_MODEL ** -0.5)
    moe_b_gu = nrm(ks[17], (DEPTH, N_EXPERTS, 2 * EXPERT_FF), 0.02)
    moe_w_down = nrm(ks[18], (DEPTH, N_EXPERTS, EXPERT_FF, D_MODEL), EXPERT_FF ** -0.5 * beta)
    moe_b_down = nrm(ks[19], (DEPTH, N_EXPERTS, D_MODEL), 0.02)
    return {"x": x, "positions": positions,
            "mla_w_in": mla_w_in, "mla_g_q": mla_g_q, "mla_g_kv": mla_g_kv,
            "mla_w_qb": mla_w_qb, "mla_w_kvb": mla_w_kvb, "mla_w_o": mla_w_o,
            "moba_w_qkv": moba_w_qkv, "moba_w_o": moba_w_o,
            "ln1_g": ln1_g, "ln1_b": ln1_b, "ln2_g": ln2_g, "ln2_b": ln2_b,
            "moe_w_router": moe_w_router, "moe_b_router": moe_b_router,
            "moe_w_gu": moe_w_gu, "moe_b_gu": moe_b_gu,
            "moe_w_down": moe_w_down, "moe_b_down": moe_b_down}


def reference(x, positions, mla_w_in, mla_g_q, mla_g_kv, mla_w_qb, mla_w_kvb, mla_w_o,
              moba_w_qkv, moba_w_o, ln1_g, ln1_b, ln2_g, ln2_b,
              moe_w_router, moe_b_router, moe_w_gu, moe_b_gu, moe_w_down, moe_b_down):
    B, S, D = x.shape
    for i in range(DEPTH):
        j = i // 2
        if i % 2 == 0:
            y = mla_attention(x, positions, mla_w_in[j], mla_g_q[j], mla_g_kv[j],
                              mla_w_qb[j], mla_w_kvb[j], mla_w_o[j])
        else:
            y = moba_attention(x, positions, moba_w_qkv[j], moba_w_o[j])
        x = layer_norm(DEEPNORM_ALPHA * x + y, ln1_g[i], ln1_b[i])
        y = moe_ffn(x.reshape(B * S, D), moe_w_router[i], moe_b_router[i], moe_w_gu[i],
                    moe_b_gu[i], moe_w_down[i], moe_b_down[i]).reshape(B, S, D)
        x = layer_norm(DEEPNORM_ALPHA * x + y, ln2_g[i], ln2_b[i])
    return x
```

```python
from contextlib import ExitStack
import os
import numpy as np
import ml_dtypes
import concourse.bass as bass
import concourse.mybir as mybir
from concourse.bass_utils import run_bass_kernel_spmd

dt = mybir.dt
F32, BF16, I32 = dt.float32, dt.bfloat16, dt.int32
ALU = mybir.AluOpType
AF = mybir.ActivationFunctionType
AX = mybir.AxisListType

D = 2048
NC_ = 16
H = 16
FF = 1024
TOPK = 4
NEG = -30000.0
LN_EPS = 1e-5
RMS_EPS = 1e-6


class Sem:
    _n = 0

    def __init__(self, h):
        self.h = h
        Sem._n += 1
        self.id = Sem._n
        self.val = 0


class Tok:
    __slots__ = ("w", "r")

    def __init__(self):
        self.w = None
        self.r = {}


class Q:
    EPOCH = 60000

    def __init__(self, K, eng, name, is_pe=False):
        self.K, self.e, self.name, self.is_pe = K, eng, name, is_pe
        self.sem = K.new_sem(name)
        self.retired = []
        self.seen = {}
        self.dma_sems = [None] * K.n_dma_sems
        self.dma_i = 0
        self.pending = False

    def wait(self, ev):
        sem, val = ev
        if self.seen.get(sem.id, 0) >= val:
            return
        self.e.wait_ge(sem.h, val)
        self.seen[sem.id] = val


class K:
    def __init__(self, nc, n_dma_sems=8):
        self.nc = nc
        self.es = ExitStack()
        self.n_dma_sems = n_dma_sems
        self.nsem = 0
        self.pe = Q(self, nc.tensor, "pe", is_pe=True)
        self.act = Q(self, nc.scalar, "act")
        self.dve = Q(self, nc.vector, "dve")
        self.pool = Q(self, nc.gpsimd, "pool")
        self.sp = Q(self, nc.sync, "sp")
        self.qs = [self.pe, self.act, self.dve, self.pool, self.sp]

    def new_sem(self, name):
        self.nsem += 1
        return Sem(self.es.enter_context(self.nc.semaphore(f"s{self.nsem}_{name}")))

    def _deps(self, q, r, w):
        evs = []
        for t in r:
            if t.w is not None:
                evs.append(t.w)
        for t in w:
            if t.w is not None:
                evs.append(t.w)
            evs.extend(t.r.values())
        for ev in evs:
            if q.is_pe and ev[0] is q.sem:
                continue
            q.wait(ev)

    def _record(self, ev, r, w):
        for t in r:
            t.r[ev[0].id] = ev
        for t in w:
            t.w = ev
            t.r = {}

    def op(self, q, fn, r=(), w=(), inc=True):
        self._deps(q, r, w)
        if q.sem.val >= Q.EPOCH and not q.pending:
            q.retired.append(q.sem)
            q.sem = self.new_sem(q.name)
        inst = fn()
        if inc:
            q.sem.val += 1
            inst.then_inc(q.sem.h, 1)
            ev = (q.sem, q.sem.val)
            q.pending = False
        else:
            ev = (q.sem, q.sem.val + 1)
            q.pending = True
        self._record(ev, r, w)
        return ev

    def dma(self, q, fn, r=(), w=()):
        self._deps(q, r, w)
        j = q.dma_i % len(q.dma_sems)
        if q.dma_sems[j] is None:
            q.dma_sems[j] = self.new_sem(f"{q.name}_d{j}")
        s = q.dma_sems[j]
        q.dma_i += 1
        if s.val > 0:
            q.wait((s, s.val))
        if s.val >= Q.EPOCH:
            q.retired.append(s)
            s = q.dma_sems[j] = self.new_sem(f"{q.name}_d{j}")
        inst = fn()
        s.val += 16
        inst.then_inc(s.h, 16)
        ev = (s, s.val)
        self._record(ev, r, w)
        return ev

    def barrier(self, only=None):
        evs = []
        for p in self.qs:
            for s in p.retired + [p.sem] + [d for d in p.dma_sems if d is not None]:
                if s.val > 0:
                    evs.append((s, s.val))
            p.retired = []
        for q in (only or self.qs):
            for ev in evs:
                if q.is_pe and ev[0] is q.sem:
                    continue
                q.wait(ev)


def _bf(a):
    return np.ascontiguousarray(a).astype(ml_dtypes.bfloat16)


def build(B, S, L, E, names):
    nc = bass.Bass("TRN2", target_bir_lowering=False)
    T = B * S
    NT = T // 128
    NB = S // 256
    NQ = S // 512
    ext = {}
    for n, (shp, dty) in names.items():
        ext[n] = nc.dram_tensor(n, list(shp), dty, kind="ExternalInput").ap()
    out = nc.dram_tensor("out", [T, D], F32, kind="ExternalOutput").ap()

    def scratch(name, shape, dtype):
        return nc.dram_tensor(name, list(shape), dtype).ap()

    XA = scratch("XA", [T, D], F32)
    X1 = scratch("X1", [T, D], F32)
    XT = scratch("XT", [D, T], BF16)
    X1T = scratch("X1T", [D, T], BF16)
    OT = scratch("OT", [D, T], BF16)
    QTn = scratch("QTn", [H, 128, T], BF16)
    QTr = scratch("QTr", [H, 64, T], BF16)
    KTn = scratch("KTn", [H, 128, T], BF16)
    KR = scratch("KR", [64, T], BF16)
    VV = scratch("VV", [T, D], BF16)
    CQ = scratch("CQ", [512, T], BF16)
    CKV = scratch("CKV", [512, T], BF16)
    CAP = cap_of(T, E)
    SU = CAP // 2
    GD = scratch("GD", [T, TOPK], F32)
    EPP = min(E, int(os.environ.get("KEPP", "8")))
    G = E // EPP
    DI = scratch("DI2", [T, G * TOPK], I32)
    XSp = [scratch(f"XS{g}", [EPP * CAP, D], F32) for g in range(G)]
    YSp = [scratch(f"YS{g}", [EPP * CAP, D], F32) for g in range(G)]
    LK = scratch("LK", [B, H, 28, S], BF16)
    RQ = scratch("RQ", [B, H, 28, S], BF16)

    k = K(nc)
    es = k.es
    _uid = [0]

    def _sbt(name, shape, dtype):
        _uid[0] += 1
        return nc.sbuf_tensor(f"{name}_u{_uid[0]}", shape, dtype)
    pe, act, dve, pool, sp = k.pe, k.act, k.dve, k.pool, k.sp

    def sbp(name, shape, dtype):
        return es.enter_context(_sbt(name, list(shape), dtype))

    PS = [es.enter_context(nc.psum_tensor(f"ps{i}", [128, 512], F32)) for i in range(8)]
    PT = [Tok() for _ in range(8)]
    identf = sbp("identf_s", [128, 128], F32)
    identb = sbp("identb_s", [128, 128], BF16)
    onesb = sbp("onesb_s", [128, 128], BF16)
    cm = sbp("cm_s", [128, 4, 512], BF16)
    c16 = sbp("c16_s", [128, 2, 16], F32)
    ustr = sbp("ustr_s", [128, 128], BF16)
    tconst = Tok()
    k.dma(sp, lambda: nc.sync.dma_start(out=identf[:], in_=ext["identf"]), w=[tconst])
    k.dma(sp, lambda: nc.sync.dma_start(out=identb[:], in_=ext["identb"]), w=[tconst])
    k.dma(sp, lambda: nc.sync.dma_start(out=onesb[:], in_=ext["onesb"]), w=[tconst])
    k.dma(sp, lambda: nc.sync.dma_start(out=cm[:], in_=ext["cm"]), w=[tconst])
    k.dma(sp, lambda: nc.sync.dma_start(out=c16[:], in_=ext["c16"]), w=[tconst])
    k.dma(sp, lambda: nc.sync.dma_start(out=ustr[:], in_=ext["ustr"]), w=[tconst])
    bnd = nc.gpsimd.alloc_register("bnd")
    nc.gpsimd.reg_mov(bnd, EPP * CAP - 1)
    k.barrier()

    def MM(ps, lhsT, rhs, start, stop, r, w, inc=True):
        k.op(pe, lambda: nc.tensor.matmul(ps, lhsT=lhsT, rhs=rhs, start=start, stop=stop), r=r, w=w, inc=inc)

    def TR(ps, in_, r, w):
        k.op(pe, lambda: nc.tensor.transpose(ps, in_, identf[:in_.shape[0], :in_.shape[0]]), r=r, w=w)

    def ACT(out_, in_, func, r, w, **kw):
        k.op(act, lambda: nc.scalar.activation(out=out_, in_=in_, func=func, **kw), r=r, w=w)

    def TT(q, out_, in0, in1, op, r, w):
        k.op(q, lambda: q.e.tensor_tensor(out=out_, in0=in0, in1=in1, op=op), r=r, w=w)

    def TS(q, out_, in0, s1, s2, op0, op1, r, w):
        if op1 is None:
            k.op(q, lambda: q.e.tensor_scalar(out=out_, in0=in0, scalar1=s1, scalar2=None, op0=op0), r=r, w=w)
        else:
            k.op(q, lambda: q.e.tensor_scalar(out=out_, in0=in0, scalar1=s1, scalar2=s2, op0=op0, op1=op1), r=r, w=w)

    def STT(q, out_, in0, scalar, in1, op0, op1, r, w):
        k.op(q, lambda: q.e.scalar_tensor_tensor(out=out_, in0=in0, scalar=scalar, in1=in1, op0=op0, op1=op1), r=r, w=w)

    def CP(q, out_, in_, r, w):
        if q is act:
            k.op(q, lambda: nc.scalar.copy(out=out_, in_=in_), r=r, w=w)
        else:
            k.op(q, lambda: q.e.tensor_copy(out=out_, in_=in_), r=r, w=w)

    def MEMSET(q, ap, val, w):
        k.op(q, lambda: q.e.memset(ap, val), w=w)

    def LD(q, out_, in_, w, r=()):
        k.dma(q, lambda: q.e.dma_start(out=out_, in_=in_), r=r, w=w)

    def ST(q, out_, in_, r):
        k.dma(q, lambda: q.e.dma_start(out=out_, in_=in_), r=r)

    class Ring:
        def __init__(self, st, name, n, shape, dtype):
            self.t = [st.enter_context(_sbt(f"{name}{i}", list(shape), dtype)) for i in range(n)]
            self.k = [Tok() for _ in range(n)]
            self.i = 0

        def next(self):
            j = self.i % len(self.t)
            self.i += 1
            return self.t[j], self.k[j]

    class PRing:
        def __init__(self, idxs):
            self.idxs = idxs
            self.i = 0

        def next(self):
            j = self.idxs[self.i % len(self.idxs)]
            self.i += 1
            return PS[j], PT[j]

    def layer_norm(st_tiles, z, tz, gt, bt, tgb, o, to):
        junk, tj, stat, tstat = st_tiles
        ACT(junk[:], z[:], AF.Copy, r=[tz], w=[tj, tstat], accum_out=stat[:, 0:1])
        ACT(junk[:], z[:], AF.Square, r=[tz], w=[tj, tstat], accum_out=stat[:, 1:2])
        TS(dve, stat[:, 2:3], stat[:, 0:1], 1.0 / D, None, ALU.mult, None, r=[tstat], w=[tstat])
        TT(dve, stat[:, 3:4], stat[:, 2:3], stat[:, 2:3], ALU.mult, r=[tstat], w=[tstat])
        STT(dve, stat[:, 4:5], stat[:, 1:2], 1.0 / D, stat[:, 3:4], ALU.mult, ALU.subtract, r=[tstat], w=[tstat])
        TS(dve, stat[:, 4:5], stat[:, 4:5], LN_EPS, None, ALU.add, None, r=[tstat], w=[tstat])
        ACT(stat[:, 5:6], stat[:, 4:5], AF.Sqrt, r=[tstat], w=[tstat])
        k.op(dve, lambda: nc.vector.reciprocal(out=stat[:, 6:7], in_=stat[:, 5:6]), r=[tstat], w=[tstat])
        TS(dve, o[:], z[:], stat[:, 2:3], stat[:, 6:7], ALU.subtract, ALU.mult, r=[tz, tstat], w=[to])
        TT(dve, o[:], o[:], gt[:], ALU.mult, r=[to, tgb], w=[to])
        TT(dve, o[:], o[:], bt[:], ALU.add, r=[to, tgb], w=[to])

    def transpose_tile(pr, x, tx, xt, txt):
        for g in range(4):
            ps, tp = pr.next()
            for j in range(4):
                c = g * 4 + j
                TR(ps[:, j * 128:(j + 1) * 128], x[:, c * 128:(c + 1) * 128], r=[tx, tconst], w=[tp])
            CP(act if g % 2 else dve, xt[:, g * 4:(g + 1) * 4, :],
               ps[:].rearrange("p (j t) -> p j t", j=4), r=[tp], w=[txt])

    for c in range(0, D, 512):
        k.dma(pool, lambda: nc.gpsimd.dma_start(out=XT[c:c + 512, :], in_=ext["xT"][c:c + 512, :]))
    with ExitStack() as st:
        COS = scratch("COS", [B, 64, S], F32)
        SIN = scratch("SIN", [B, 64, S], F32)
        pi_ = st.enter_context(_sbt("pos_i", [64, S], I32))
        pf = st.enter_context(_sbt("pos_f", [64, S], F32))
        a1 = st.enter_context(_sbt("ang1", [64, S], F32))
        a2 = st.enter_context(_sbt("ang2", [64, S], F32))
        invf = st.enter_context(_sbt("invf_s", [64, 1], F32))
        rb = st.enter_context(_sbt("rb", [3, S], F32))
        ra = st.enter_context(_sbt("ra", [3, S], F32))
        rbb = st.enter_context(_sbt("rbb", [3, 2, S], BF16))
        rab = st.enter_context(_sbt("rab", [3, 2, S], BF16))
        tq_ = st.enter_context(_sbt("tq_", [64, S], F32))
        ri = st.enter_context(_sbt("ri", [3, S], I32))
        tp_, tf_, t1_, t2_, tv_, tr_, ttq = Tok(), Tok(), Tok(), Tok(), Tok(), Tok(), Tok()
        LD(sp, invf[:], ext["invf"], w=[tv_])
        k.dma(pool, lambda: nc.gpsimd.dma_start(out=LK.rearrange("b h r s -> (b h r) s"), in_=ext["lkc"].rearrange("b h r s -> (b h r) s")))
        k.dma(pool, lambda: nc.gpsimd.dma_start(out=RQ.rearrange("b h r s -> (b h r) s"), in_=ext["rqc"].rearrange("b h r s -> (b h r) s")))
        k.barrier()
        for b in range(B):
            LD(sp, pi_[:], ext["pos"][b:b + 1, :].partition_broadcast(64), w=[tp_])
            CP(dve, pf[:], pi_[:], r=[tp_], w=[tf_])
            TS(dve, a1[:], pf[:], invf[:, 0:1], None, ALU.mult, None, r=[tf_, tv_], w=[t1_])
            TS(dve, a2[:], a1[:], float(np.pi / 2), None, ALU.add, None, r=[t1_], w=[t2_])
            TWO_PI = float(2 * np.pi)
            for a, ta in ((a1, t1_), (a2, t2_)):
                TS(dve, tq_[:], a[:], 1.0 / TWO_PI, None, ALU.mult, None, r=[ta], w=[ttq])
                CP(dve, pi_[:], tq_[:], r=[ttq, tp_], w=[tp_])
                CP(dve, tq_[:], pi_[:], r=[tp_], w=[ttq])
                STT(dve, a[:], tq_[:], -TWO_PI, a[:], ALU.mult, ALU.add, r=[ttq, ta], w=[ta])
                TS(dve, tq_[:], a[:], float(np.pi), TWO_PI, ALU.is_gt, ALU.mult, r=[ta], w=[ttq])
                TT(dve, a[:], a[:], tq_[:], ALU.subtract, r=[ta, ttq], w=[ta])
                TS(dve, tq_[:], a[:], float(-np.pi), TWO_PI, ALU.is_lt, ALU.mult, r=[ta], w=[ttq])
                TT(dve, a[:], a[:], tq_[:], ALU.add, r=[ta, ttq], w=[ta])
                TS(dve, a[:], a[:], float(np.pi), float(-np.pi), ALU.min, ALU.max, r=[ta], w=[ta])
                ACT(a[:], a[:], AF.Sin, r=[ta], w=[ta])
            TS(dve, a1[0:32, :], a1[0:32, :], -1.0, None, ALU.mult, None, r=[t1_], w=[t1_])
            ST(sp, COS[b], a2[:], r=[t2_])
            ST(sp, SIN[b], a1[:], r=[t1_])
            TS(dve, rb[:], pf[0:3, :], pf[0:3, 0:1], None, ALU.subtract, None, r=[tf_], w=[tr_])
            TS(dve, ra[:], rb[:], 1.0 / 64, None, ALU.mult, None, r=[tr_], w=[tr_])
            CP(dve, ri[:], ra[:], r=[tr_], w=[tr_])
            CP(dve, ra[:], ri[:], r=[tr_], w=[tr_])
            TS(dve, ra[:], ra[:], 64.0, None, ALU.mult, None, r=[tr_], w=[tr_])
            TT(dve, rb[:], rb[:], ra[:], ALU.subtract, r=[tr_], w=[tr_])
            CP(dve, rab[:, 0, :], ra[:], r=[tr_], w=[tr_])
            CP(dve, rbb[:, 0, :], rb[:], r=[tr_], w=[tr_])
            TS(dve, rab[:, 1, :], ra[:], -1.0, None, ALU.mult, None, r=[tr_], w=[tr_])
            TS(dve, rbb[:, 1, :], rb[:], -1.0, None, ALU.mult, None, r=[tr_], w=[tr_])
            for h in range(H):
                ST(sp, LK[b, h, 16:19, :], rab[:, 0, :], r=[tr_])
                ST(sp, LK[b, h, 19:22, :], rbb[:, 0, :], r=[tr_])
                ST(sp, RQ[b, h, 22:25, :], rab[:, 1, :], r=[tr_])
                ST(sp, RQ[b, h, 25:28, :], rbb[:, 1, :], r=[tr_])
        k.barrier()

    def mla_proj(j, Xin_T):
        sc = float((128 + 64) ** -0.5)
        with ExitStack() as st:
            win = st.enter_context(_sbt("win", [128, NC_, 1088], BF16))
            wins = st.enter_context(_sbt("wins", [128, NC_, 64], BF16))
            gq = st.enter_context(_sbt("gq", [128, 4], F32))
            gkv = st.enter_context(_sbt("gkv", [128, 4], F32))
            tw = Tok()
            k.dma(pool, lambda: nc.gpsimd.dma_start(out=win[:], in_=ext[f"mla_w_in{j}"].rearrange("(c p) n -> p c n", p=128)), w=[tw])
            k.dma(pool, lambda: nc.gpsimd.dma_start(out=wins[:], in_=ext[f"mla_w_in_sw{j}"].rearrange("(c p) n -> p c n", p=128)), w=[tw])
            LD(sp, gq[:], ext[f"mla_g_q{j}"], w=[tw])
            LD(sp, gkv[:], ext[f"mla_g_kv{j}"], w=[tw])
            xr = Ring(st, "xr", 2, [128, NC_, 512], BF16)
            cf = Ring(st, "cf", 2, [128, 4, 512], F32)
            sq = Ring(st, "sq", 2, [128, 512], BF16)
            rs = Ring(st, "rs", 2, [128, 512], F32)
            cn = Ring(st, "cn", 2, [128, 4, 512], BF16)
            cs = Ring(st, "cs", 2, [64, 512], F32)
            t1r = Ring(st, "t1r", 2, [64, 512], F32)
            kro = Ring(st, "kro", 2, [64, 512], BF16)
            pa = PRing([0, 1, 2, 3])
            pss = PRing([4, 5])
            pk = PRing([6, 7])
            for tt in range(T // 512):
                b = (tt * 512) // S
                s0 = tt * 512 - b * S
                x_, tx = xr.next()
                LD(sp, x_[:], Xin_T.rearrange("(c p) t -> p c t", p=128)[:, :, tt * 512:(tt + 1) * 512], w=[tx])
                cos_, tcs = cs.next()
                sin_, tsn = cs.next()
                LD(sp, cos_[:], COS[b, :, s0:s0 + 512], w=[tcs])
                LD(sp, sin_[:], SIN[b, :, s0:s0 + 512], w=[tsn])
                for which, goff, gvec, dst in ((0, 0, gq, CQ), (1, 512, gkv, CKV)):
                    c_, tc = cf.next()
                    ss, tss = pss.next()
                    for ch in range(4):
                        ps, tp = pa.next()
                        for c in range(NC_):
                            MM(ps[:], win[:, c, goff + ch * 128: goff + (ch + 1) * 128], x_[:, c, :], c == 0, c == NC_ - 1, r=[tw, tx], w=[tp])
                        CP(act, c_[:, ch, :], ps[:], r=[tp], w=[tc])
                        s_, ts_ = sq.next()
                        ACT(s_[:], ps[:], AF.Square, r=[tp], w=[ts_])
                        MM(ss[:], onesb[:], s_[:], ch == 0, ch == 3, r=[ts_, tconst], w=[tss])
                    r_, trs = rs.next()
                    TS(dve, r_[:], ss[:], 1.0 / 512, RMS_EPS, ALU.mult, ALU.add, r=[tss], w=[trs])
                    ACT(r_[:], r_[:], AF.Sqrt, r=[trs], w=[trs])
                    k.op(dve, lambda: nc.vector.reciprocal(out=r_[:], in_=r_[:]), r=[trs], w=[trs])
                    n_, tn = cn.next()
                    for ch in range(4):
                        STT(dve, n_[:, ch, :], c_[:, ch, :], gvec[:, ch:ch + 1], r_[:], ALU.mult, ALU.mult, r=[tc, trs, tw], w=[tn])
                    ST(sp, dst.rearrange("(c p) t -> p c t", p=128)[:, :, tt * 512:(tt + 1) * 512], n_[:], r=[tn])
                p1, tp1 = pk.next()
                p2, tp2 = pk.next()
                for c in range(NC_):
                    MM(p1[0:64, :], win[:, c, 1024:1088], x_[:, c, :], c == 0, c == NC_ - 1, r=[tw, tx], w=[tp1])
                for c in range(NC_):
                    MM(p2[0:64, :], wins[:, c, :], x_[:, c, :], c == 0, c == NC_ - 1, r=[tw, tx], w=[tp2])
                u1, tu1 = t1r.next()
                u2, tu2 = t1r.next()
                TT(dve, u1[:], p1[0:64, :], cos_[:], ALU.mult, r=[tp1, tcs], w=[tu1])
                TT(dve, u2[:], p2[0:64, :], sin_[:], ALU.mult, r=[tp2, tsn], w=[tu2])
                ko, tko = kro.next()
                TT(dve, ko[:], u1[:], u2[:], ALU.add, r=[tu1, tu2], w=[tko])
                ST(sp, KR[:, tt * 512:(tt + 1) * 512], ko[:], r=[tko])
            k.barrier()
        with ExitStack() as st:
            wq = st.enter_context(_sbt("wq", [128, 4, H * 192], BF16))
            wqs = st.enter_context(_sbt("wqs", [128, 4, H * 64], BF16))
            wkv = st.enter_context(_sbt("wkv", [128, 4, H * 256], BF16))
            tw = Tok()
            k.dma(pool, lambda: nc.gpsimd.dma_start(out=wq[:], in_=ext[f"mla_w_qb{j}"].rearrange("(c p) n -> p c n", p=128)), w=[tw])
            k.dma(pool, lambda: nc.gpsimd.dma_start(out=wqs[:], in_=ext[f"mla_w_qb_sw{j}"].rearrange("(c p) n -> p c n", p=128)), w=[tw])
            k.dma(pool, lambda: nc.gpsimd.dma_start(out=wkv[:], in_=ext[f"mla_w_kvb{j}"].rearrange("(c p) n -> p c n", p=128)), w=[tw])
            cqr = Ring(st, "cqr", 2, [128, 4, 512], BF16)
            ckr = Ring(st, "ckr", 2, [128, 4, 512], BF16)
            cs = Ring(st, "cs2", 4, [64, 512], F32)
            qn = Ring(st, "qn", 3, [128, 512], BF16)
            qr = Ring(st, "qr", 3, [64, 512], BF16)
            kn = Ring(st, "kn", 3, [128, 512], BF16)
            vt = Ring(st, "vt", 2, [128, D], BF16)
            u1r = Ring(st, "u1r", 3, [64, 512], F32)
            u2r = Ring(st, "u2r", 3, [64, 512], F32)
            pa = PRing([0, 1, 2])
            pb = PRing([3, 4])
            pc = PRing([5, 6, 7])
            for tt in range(T // 512):
                b = (tt * 512) // S
                s0 = tt * 512 - b * S
                cq_, tcq = cqr.next()
                ck_, tck = ckr.next()
                LD(sp, cq_[:], CQ.rearrange("(c p) t -> p c t", p=128)[:, :, tt * 512:(tt + 1) * 512], w=[tcq])
                LD(sp, ck_[:], CKV.rearrange("(c p) t -> p c t", p=128)[:, :, tt * 512:(tt + 1) * 512], w=[tck])
                cos_, tcs = cs.next()
                sin_, tsn = cs.next()
                LD(sp, cos_[:], COS[b, :, s0:s0 + 512], w=[tcs])
                LD(sp, sin_[:], SIN[b, :, s0:s0 + 512], w=[tsn])
                for h in range(H):
                    qn_, tqn = qn.next()
                    qr_, tqr = qr.next()
                    kn_, tkn = kn.next()
                    ps, tp = pa.next()
                    for c in range(4):
                        MM(ps[:], wq[:, c, h * 192:h * 192 + 128], cq_[:, c, :], c == 0, c == 3, r=[tw, tcq], w=[tp])
                    ACT(qn_[:], ps[:], AF.Copy, r=[tp], w=[tqn], scale=sc)
                    ST(sp, QTn[h, :, tt * 512:(tt + 1) * 512], qn_[:], r=[tqn])
                    p1, tp1 = pb.next()
                    p2, tp2 = pb.next()
                    for c in range(4):
                        MM(p1[0:64, :], wq[:, c, h * 192 + 128:h * 192 + 192], cq_[:, c, :], c == 0, c == 3, r=[tw, tcq], w=[tp1])
                    for c in range(4):
                        MM(p2[0:64, :], wqs[:, c, h * 64:(h + 1) * 64], cq_[:, c, :], c == 0, c == 3, r=[tw, tcq], w=[tp2])
                    u1, tu1 = u1r.next()
                    u2, tu2 = u2r.next()
                    STT(dve, u1[:], p1[0:64, :], sc, cos_[:], ALU.mult, ALU.mult, r=[tp1, tcs], w=[tu1])
                    STT(dve, u2[:], p2[0:64, :], sc, sin_[:], ALU.mult, ALU.mult, r=[tp2, tsn], w=[tu2])
                    TT(pool, qr_[:], u1[:], u2[:], ALU.add, r=[tu1, tu2], w=[tqr])
                    ST(sp, QTr[h, :, tt * 512:(tt + 1) * 512], qr_[:], r=[tqr])
                    ps, tp = pc.next()
                    for c in range(4):
                        MM(ps[:], wkv[:, c, h * 256:h * 256 + 128], ck_[:, c, :], c == 0, c == 3, r=[tw, tck], w=[tp])
                    CP(act, kn_[:], ps[:], r=[tp], w=[tkn])
                    ST(sp, KTn[h, :, tt * 512:(tt + 1) * 512], kn_[:], r=[tkn])
                for sub in range(4):
                    v_, tv = vt.next()
                    for hg in range(4):
                        ps, tp = pc.next()
                        for c in range(4):
                            MM(ps[:].rearrange("p (h d) -> p h d", h=4), ck_[:, c, sub * 128:(sub + 1) * 128],
                               wkv[:, c, :].rearrange("p (h j) -> p h j", j=256)[:, hg * 4:(hg + 1) * 4, 128:256],
                               c == 0, c == 3, r=[tw, tck], w=[tp])
                        CP(dve if hg % 2 else act, v_[:, hg * 512:(hg + 1) * 512], ps[:], r=[tp], w=[tv])
                    ST(sp, VV[tt * 512 + sub * 128: tt * 512 + (sub + 1) * 128, :], v_[:], r=[tv])
            k.barrier()

    def moba_proj(j, Xin_T):
        sc = float(128 ** -0.5)
        TH = min(T, 2048)
        wsrc = ext[f"moba_w_qkv{j}"].rearrange("(c p) n -> p c n", p=128)
        with ExitStack() as st:
            xs = st.enter_context(_sbt("xs", [128, NC_, TH], BF16))
            km = st.enter_context(_sbt("km", [128, B, H, NB], F32))
            kmb = st.enter_context(_sbt("kmb", [128, B, H, NB], BF16))
            tkm = Tok()
            tkmb = Tok()
            txs = Tok()
            wr = Ring(st, "wr", 3, [128, NC_, 512], BF16)
            ob = Ring(st, "ob", 3, [128, 512], BF16)
            vt = Ring(st, "vt", 2, [128, 512], BF16)
            gmr = Ring(st, "gmr", 3, [128, 16], F32)
            m8r = Ring(st, "m8r", 3, [128, 8], F32)
            mbr = Ring(st, "mbr", 3, [128, 16], F32)
            mtr = Ring(st, "mtr", 3, [16, 512], BF16)
            pa = PRing([0, 1, 2, 3])
            pg = PRing([4, 5])
            pt_ = PRing([6, 7])
            for th in range(T // TH):
                t0 = th * TH
                kmb_done = [False]
                for q4 in range(TH // 512):
                    LD(sp, xs[:, :, q4 * 512:(q4 + 1) * 512],
                       Xin_T.rearrange("(c p) t -> p c t", p=128)[:, :, t0 + q4 * 512: t0 + (q4 + 1) * 512], w=[txs])
                for slab in ([int(v) for v in os.environ['KSLABS'].split(',')] if os.environ.get('KSLABS') else [4, 5, 6, 7, 8, 9, 10, 11, 0, 1, 2, 3]):
                    w_, tw = wr.next()
                    k.dma(pool, lambda: nc.gpsimd.dma_start(out=w_[:], in_=wsrc[:, :, slab * 512:(slab + 1) * 512]), w=[tw])
                    kind = slab // 4
                    if kind == 0 and not kmb_done[0]:
                        CP(dve, kmb[:].rearrange("p b h m -> p (b h m)"), km[:].rearrange("p b h m -> p (b h m)"), r=[tkm], w=[tkmb])
                        kmb_done[0] = True
                    if kind == 2:
                        for sub in range(TH // 128):
                            ps, tp = pa.next()
                            for c in range(NC_):
                                MM(ps[:], xs[:, c, sub * 128:(sub + 1) * 128], w_[:, c, :], c == 0, c == NC_ - 1, r=[txs, tw], w=[tp])
                            v_, tv = vt.next()
                            CP(act if sub % 2 else dve, v_[:], ps[:], r=[tp], w=[tv])
                            ST(sp, VV[t0 + sub * 128: t0 + (sub + 1) * 128, (slab - 8) * 512:(slab - 7) * 512], v_[:], r=[tv])
                        continue
                    KM = int(os.environ.get("KMODE", "9"))
                    for hh in range(4 if KM > 0 else 0):
                        h = (slab % 4) * 4 + hh
                        for tq in range(TH // 512):
                            tg = t0 + tq * 512
                            b = tg // S
                            s0 = tg - b * S
                            ps, tp = pa.next()
                            for c in range(NC_):
                                MM(ps[:], w_[:, c, hh * 128:(hh + 1) * 128], xs[:, c, tq * 512:(tq + 1) * 512], c == 0, c == NC_ - 1, r=[txs, tw], w=[tp])
                            o_, to = ob.next()
                            if kind == 1:
                                blk0 = s0 // 256
                                ACT(o_[:, 0:256], ps[:, 0:256], AF.Copy, r=[tp], w=[to, tkm], accum_out=km[:, b, h, blk0:blk0 + 1])
                                ACT(o_[:, 256:512], ps[:, 256:512], AF.Copy, r=[tp], w=[to, tkm], accum_out=km[:, b, h, blk0 + 1:blk0 + 2])
                                ST(sp, KTn[h, :, tg:tg + 512], o_[:], r=[to])
                            else:
                                ACT(o_[:], ps[:], AF.Copy, r=[tp], w=[to], scale=sc)
                                ST(sp, QTn[h, :, tg:tg + 512], o_[:], r=[to])
                                mt, tmt = mtr.next()
                                for qs in range(4 if os.environ.get("KNOGATE") is None else 0):
                                    qb = (s0 + qs * 128) // 256
                                    mb, tmb = mbr.next()
                                    CP(dve, mb[:], c16[:, 0, :], r=[tconst], w=[tmb])
                                    if qb > 0:
                                        pgs, tpg = pg.next()
                                        MM(pgs[:, 0:NB], o_[:, qs * 128:(qs + 1) * 128], kmb[:, b, h, :], True, True, r=[to, tkmb], w=[tpg])
                                        gm, tgm = gmr.next()
                                        CP(dve, gm[:], c16[:, 1, :], r=[tconst], w=[tgm])
                                        CP(dve, gm[:, 0:qb], pgs[:, 0:qb], r=[tpg], w=[tgm])
                                        m8, tm8 = m8r.next()
                                        k.op(dve, lambda: nc.vector.max(out=m8[:], in_=gm[:]), r=[tgm], w=[tm8])
                                        TS(dve, mb[:, 0:qb], gm[:, 0:qb], m8[:, 2:3], NEG, ALU.is_lt, ALU.mult, r=[tgm, tm8], w=[tmb])
                                    ptp, tpt = pt_.next()
                                    TR(ptp[0:16, 0:128], mb[:], r=[tmb, tconst], w=[tpt])
                                    CP(act, mt[:, qs * 128:(qs + 1) * 128], ptp[0:16, 0:128], r=[tpt], w=[tmt])
                                ST(sp, RQ[b, h, 0:16, s0:s0 + 512], mt[:], r=[tmt])
            k.barrier()

    def attention(is_mla):
        with ExitStack() as st:
            kt = Ring(st, "kt", 2, [128, S], BF16)
            qt = Ring(st, "qt", 2, [128, S], BF16)
            vv = Ring(st, "vv", 2, [128, S // 128, 128], BF16)
            if is_mla:
                krs = st.enter_context(_sbt("krs", [64, S], BF16))
                tkr = Tok()
                qrr = Ring(st, "qrr", 2, [64, S], BF16)
            else:
                lkr = Ring(st, "lkr", 2, [28, S], BF16)
                rqr = Ring(st, "rqr", 2, [28, S], BF16)
            pr = Ring(st, "pr", 3, [128, 512], BF16)
            rl = Ring(st, "rl", 2, [128, 512], F32)
            on = Ring(st, "on", 2, [128, 512], BF16)
            psS = PRing([0, 1, 2])
            psO = PRing([3, 4])
            psL = PRing([5, 6])
            for b in range(B):
                if is_mla:
                    LD(sp, krs[:], KR[:, b * S:(b + 1) * S], w=[tkr])
                for h in range(H):
                    k_, tk = kt.next()
                    q_, tq = qt.next()
                    v_, tv = vv.next()
                    LD(sp, k_[:], KTn[h, :, b * S:(b + 1) * S], w=[tk])
                    LD(sp, q_[:], QTn[h, :, b * S:(b + 1) * S], w=[tq])
                    LD(sp, v_[:], VV[b * S:(b + 1) * S, h * 128:(h + 1) * 128].rearrange("(t p) d -> p t d", p=128), w=[tv])
                    if is_mla:
                        q2, tq2 = qrr.next()
                        LD(sp, q2[:], QTr[h, :, b * S:(b + 1) * S], w=[tq2])
                    else:
                        lk, tlk = lkr.next()
                        rq, trq = rqr.next()
                        LD(sp, lk[:], LK[b, h], w=[tlk])
                        LD(sp, rq[:], RQ[b, h], w=[trq])
                    for qi in range(NQ):
                        po, tpo = psO.next()
                        pl, tpl = psL.next()
                        nk = 4 * (qi + 1)
                        qsl = slice(qi * 512, (qi + 1) * 512)
                        for ki in range(nk):
                            ksl = slice(ki * 128, (ki + 1) * 128)
                            diag = ki >= 4 * qi
                            ps, tps = psS.next()
                            MM(ps[:], k_[:, ksl], q_[:, qsl], True, False, r=[tk, tq], w=[tps])
                            if is_mla:
                                MM(ps[:], krs[:, ksl], q2[:, qsl], False, not diag, r=[tkr, tq2], w=[tps])
                            else:
                                MM(ps[:], lk[:, ksl], rq[:, qsl], False, not diag, r=[tlk, trq], w=[tps])
                            if diag:
                                MM(ps[:], identb[:], cm[:, ki - 4 * qi, :], False, True, r=[tconst], w=[tps])
                            p_, tp = pr.next()
                            ACT(p_[:], ps[:], AF.Exp, r=[tps], w=[tp])
                            MM(po[:], v_[:, ki, :], p_[:], ki == 0, ki == nk - 1, r=[tv, tp], w=[tpo])
                            MM(pl[:], onesb[:], p_[:], ki == 0, ki == nk - 1, r=[tconst, tp], w=[tpl])
                        r_, tr = rl.next()
                        k.op(dve, lambda: nc.vector.reciprocal(out=r_[:], in_=pl[:]), r=[tpl], w=[tr])
                        o_, to = on.next()
                        TT(dve, o_[:], po[:], r_[:], ALU.mult, r=[tpo, tr], w=[to])
                        ST(sp, OT[h * 128:(h + 1) * 128, b * S + qi * 512: b * S + (qi + 1) * 512], o_[:], r=[to])
            k.barrier()

    def post_attn(li, wo_ap, Xin):
        alpha = float((2.0 * L) ** 0.25)
        with ExitStack() as st:
            wo = st.enter_context(_sbt("wo", [128, NC_, D], BF16))
            wrt = st.enter_context(_sbt("wrt", [128, NC_, E], BF16))
            brt = st.enter_context(_sbt("brt", [1, E], BF16))
            gt = st.enter_context(_sbt("lg", [128, D], F32))
            bt = st.enter_context(_sbt("lb", [128, D], F32))
            tw = Tok()
            for c4 in range(4):
                k.dma(pool, lambda: nc.gpsimd.dma_start(out=wo[:, c4 * 4:(c4 + 1) * 4, :], in_=wo_ap.rearrange("(c p) n -> p c n", p=128)[:, c4 * 4:(c4 + 1) * 4, :]), w=[tw])
            k.dma(pool, lambda: nc.gpsimd.dma_start(out=wrt[:], in_=ext[f"moe_w_router{li}"].rearrange("(c p) n -> p c n", p=128)), w=[tw])
            k.dma(pool, lambda: nc.gpsimd.dma_start(out=brt[:], in_=ext[f"moe_b_router{li}"]), w=[tw])
            LD(act, gt[:], ext[f"ln1_g{li}"].partition_broadcast(128), w=[tw])
            LD(act, bt[:], ext[f"ln1_b{li}"].partition_broadcast(128), w=[tw])
            otr = Ring(st, "otr", 2, [128, NC_, 128], BF16)
            xr = Ring(st, "xr", 2, [128, D], F32)
            zr = Ring(st, "zr", 2, [128, D], F32)
            x1r = Ring(st, "x1r", 2, [128, D], F32)
            xtr = Ring(st, "xtr", 2, [128, NC_, 128], BF16)
            junk = st.enter_context(_sbt("junk", [128, D], BF16))
            stat = st.enter_context(_sbt("stat", [128, 8], F32))
            tj, tstat = Tok(), Tok()
            lgr = Ring(st, "lgr", 2, [128, E], F32)
            exr = Ring(st, "exr", 2, [128, E], F32)
            mkr = Ring(st, "mkr", 2, [128, E], BF16)
            g4r = Ring(st, "g4r", 3, [128, TOPK], F32)
            dsr = Ring(st, "dsr", 2, [128, E], F32)
            d4r = Ring(st, "d4r", 2, [128, TOPK], F32)
            dir_ = Ring(st, "dir", 3, [128, G, TOPK], I32)
            dgr = Ring(st, "dgr", 2, [128, 2, TOPK], F32)
            basecap = st.enter_context(_sbt("basecap", [128, E], F32))
            tbase = Tok()
            LD(sp, basecap[:], ext["ecap"], w=[tbase])
            m8r = Ring(st, "m8r", 2, [128, 8], F32)
            smr = Ring(st, "smr", 2, [128, 4], F32)
            py = PRing([0, 1, 2, 3])
            ptr = PRing([4, 5])
            pl = PRing([6, 7])
            for ti in range(NT):
                tsl = slice(ti * 128, (ti + 1) * 128)
                o_, to = otr.next()
                LD(sp, o_[:], OT.rearrange("(c p) t -> p c t", p=128)[:, :, tsl], w=[to])
                x_, tx = xr.next()
                LD(sp, x_[:], Xin[tsl, :], w=[tx])
                z_, tz = zr.next()
                for n4 in range(4):
                    ps, tp = py.next()
                    for c in range(NC_):
                        MM(ps[:], o_[:, c, :], wo[:, c, n4 * 512:(n4 + 1) * 512], c == 0, c == NC_ - 1, r=[to, tw], w=[tp])
                    STT(dve, z_[:, n4 * 512:(n4 + 1) * 512], x_[:, n4 * 512:(n4 + 1) * 512], alpha, ps[:], ALU.mult, ALU.add, r=[tx, tp], w=[tz])
                x1, tx1 = x1r.next()
                layer_norm((junk, tj, stat, tstat), z_, tz, gt, bt, tw, x1, tx1)
                ST(sp, X1[tsl, :], x1[:], r=[tx1])
                xt_, txt = xtr.next()
                transpose_tile(ptr, x1, tx1, xt_, txt)
                ST(sp, X1T.rearrange("(c p) t -> p c t", p=128)[:, :, tsl], xt_[:], r=[txt])
                ps, tp = pl.next()
                for c in range(NC_):
                    MM(ps[:, 0:E], xt_[:, c, :], wrt[:, c, :], c == 0, False, r=[txt, tw], w=[tp])
                MM(ps[:, 0:E], onesb[0:1, :], brt[:], False, True, r=[tconst, tw], w=[tp])
                lg, tlg = lgr.next()
                CP(dve, lg[:], ps[:, 0:E], r=[tp], w=[tlg])
                m8, tm8 = m8r.next()
                k.op(dve, lambda: nc.vector.max(out=m8[:], in_=lg[:]), r=[tlg], w=[tm8])
                sm, tsm = smr.next()
                TS(dve, sm[:, 0:1], m8[:, 0:1], -1.0, None, ALU.mult, None, r=[tm8], w=[tsm])
                g4, tg4 = g4r.next()
                ACT(g4[:], m8[:, 0:TOPK], AF.Exp, r=[tm8, tsm], w=[tg4], bias=sm[:, 0:1], scale=1.0)
                k.op(dve, lambda: nc.vector.tensor_reduce(out=sm[:, 1:2], in_=g4[:], axis=AX.X, op=ALU.add), r=[tg4], w=[tsm])
                k.op(dve, lambda: nc.vector.reciprocal(out=sm[:, 2:3], in_=sm[:, 1:2]), r=[tsm], w=[tsm])
                TS(dve, g4[:], g4[:], sm[:, 2:3], None, ALU.mult, None, r=[tg4, tsm], w=[tg4])
                ST(sp, GD[tsl, :], g4[:], r=[tg4])
                mk, tmk = mkr.next()
                TS(dve, mk[:], lg[:], m8[:, TOPK - 1:TOPK], None, ALU.is_ge, None, r=[tlg, tm8], w=[tmk])
                pp, tpp = pl.next()
                MM(pp[:, 0:E], ustr[:], mk[:], True, True, r=[tconst, tmk], w=[tpp])
                pc, tpc = pl.next()
                MM(pc[:, 0:E], onesb[:], mk[:], True, True, r=[tconst, tmk], w=[tpc])
                ds, tds = dsr.next()
                TT(dve, ds[:], pp[:, 0:E], basecap[:], ALU.add, r=[tpp, tbase], w=[tds])
                TT(dve, basecap[:], basecap[:], pc[:, 0:E], ALU.add, r=[tpc, tbase], w=[tbase])
                d4, td4 = d4r.next()
                ex, tex = exr.next()
                for kk_ in range(TOPK):
                    k.op(dve, lambda: nc.vector.scalar_tensor_tensor(out=ex[:], in0=lg[:], scalar=m8[:, kk_:kk_ + 1], in1=ds[:],
                                                                     op0=ALU.is_equal, op1=ALU.mult, accum_out=d4[:, kk_:kk_ + 1]),
                         r=[tlg, tm8, tds], w=[tex, td4])
                di, tdi = dir_.next()
                for g in range(G):
                    if G == 1:
                        CP(dve, di[:, 0, :], d4[:], r=[td4], w=[tdi])
                    else:
                        dg, tdg = dgr.next()
                        TS(dve, dg[:, 0, :], d4[:], float(g * EPP * CAP), None, ALU.subtract, None, r=[td4], w=[tdg])
                        TS(dve, dg[:, 1, :], dg[:, 0, :], 0.0, 1.0e9, ALU.is_lt, ALU.mult, r=[tdg], w=[tdg])
                        TT(dve, dg[:, 0, :], dg[:, 0, :], dg[:, 1, :], ALU.add, r=[tdg], w=[tdg])
                        CP(dve, di[:, g, :], dg[:, 0, :], r=[tdg], w=[tdi])
                ST(sp, DI[tsl, :], di[:].rearrange("p g k -> p (g k)"), r=[tdi])
                for g in range(G):
                    for kk_ in range(TOPK):
                        k.dma(pool, lambda: nc.gpsimd.indirect_dma_start(out=XSp[g][:, :], out_offset=bass.IndirectOffsetOnAxis(ap=di[:, g, kk_:kk_ + 1], axis=0),
                                                                        in_=x1[:, :], in_offset=None, bounds_check=bnd, oob_is_err=False),
                              r=[tx1, tdi])
            k.barrier()

    def moe(li, Xout, XoutT, last):
        alpha = float((2.0 * L) ** 0.25)
        NTS = [(o, min(512, SU - o)) for o in range(0, SU, 512)]
        with ExitStack() as st:
            bgu = st.enter_context(_sbt("bgu", [128, E, 16], F32))
            tw = Tok()
            LD(sp, bgu[:], ext[f"moe_b_gu_t{li}"], w=[tw])
            xsT = st.enter_context(_sbt("xsT", [128, NC_, SU], BF16))
            txs = Tok()
            aT = st.enter_context(_sbt("aT", [128, 8, SU], BF16))
            ta = Tok()
            rows = Ring(st, "rows", 2, [128, D], F32)
            wg = Ring(st, "wg", 4, [128, NC_, 256], BF16)
            wd = Ring(st, "wd", 3, [128, 8, 512], BF16)
            bdr = Ring(st, "bdr", 2, [1, D], BF16)
            yr = Ring(st, "yr", 3, [128, 512], F32)
            gg = Ring(st, "gg", 2, [128, 512], F32)
            sg = Ring(st, "sg", 2, [128, 512], F32)
            ll = Ring(st, "ll", 2, [128, 512], F32)
            ph = PRing([0, 1, 2, 3])
            py = PRing([4, 5])
            ptr = PRing([6, 7])
            for e in range(E):
                wsrc = ext[f"wgu_{li}_{e}"].rearrange("(c p) f -> p c f", p=128)
                wdsrc = ext[f"wdn_{li}_{e}"].rearrange("(c p) n -> p c n", p=128)
                for u in range(CAP // SU):
                    r0 = (e % EPP) * CAP + u * SU
                    XS, YS = XSp[e // EPP], YSp[e // EPP]
                    bd, tbd = bdr.next()
                    k.dma(pool, lambda: nc.gpsimd.dma_start(out=bd[:], in_=ext[f"moe_b_down{li}"][e:e + 1, :]), w=[tbd])
                    for s_ in range(SU // 128):
                        rw, trw = rows.next()
                        LD(sp, rw[:], XS[r0 + s_ * 128: r0 + (s_ + 1) * 128, :], w=[trw])
                        for g in range(4):
                            ps, tp = ptr.next()
                            for j4 in range(4):
                                c = g * 4 + j4
                                TR(ps[:, j4 * 128:(j4 + 1) * 128], rw[:, c * 128:(c + 1) * 128], r=[trw, tconst], w=[tp])
                            CP(act if g % 2 else dve, xsT[:, g * 4:(g + 1) * 4, s_ * 128:(s_ + 1) * 128],
                               ps[:].rearrange("p (j t) -> p j t", j=4), r=[tp], w=[txs])
                    for fc in range(8):
                        w_, twg = wg.next()
                        k.dma(pool, lambda: nc.gpsimd.dma_start(out=w_[:, :, 0:128], in_=wsrc[:, :, fc * 128:(fc + 1) * 128]), w=[twg])
                        k.dma(pool, lambda: nc.gpsimd.dma_start(out=w_[:, :, 128:256], in_=wsrc[:, :, FF + fc * 128:FF + (fc + 1) * 128]), w=[twg])
                        for (o, n) in NTS:
                            pg_, tpg = ph.next()
                            pl_, tpl = ph.next()
                            for c in range(NC_):
                                MM(pg_[:, 0:n], w_[:, c, 0:128], xsT[:, c, o:o + n], c == 0, c == NC_ - 1, r=[twg, txs], w=[tpg], inc=(c == NC_ - 1))
                            for c in range(NC_):
                                MM(pl_[:, 0:n], w_[:, c, 128:256], xsT[:, c, o:o + n], c == 0, c == NC_ - 1, r=[twg, txs], w=[tpl], inc=(c == NC_ - 1))
                            g_, tg = gg.next()
                            s_t, ts = sg.next()
                            l_, tl = ll.next()
                            TS(dve, g_[:, 0:n], pg_[:, 0:n], bgu[:, e, fc:fc + 1], 7.0, ALU.add, ALU.min, r=[tpg, tw], w=[tg])
                            ACT(s_t[:, 0:n], g_[:, 0:n], AF.Sigmoid, r=[tg], w=[ts], scale=1.702)
                            TS(dve, l_[:, 0:n], pl_[:, 0:n], bgu[:, e, 8 + fc:9 + fc], 7.0, ALU.add, ALU.min, r=[tpl, tw], w=[tl])
                            TS(pool, l_[:, 0:n], l_[:, 0:n], -7.0, 1.0, ALU.max, ALU.add, r=[tl], w=[tl])
                            TT(pool, g_[:, 0:n], g_[:, 0:n], s_t[:, 0:n], ALU.mult, r=[tg, ts], w=[tg])
                            TT(dve, aT[:, fc, o:o + n], g_[:, 0:n], l_[:, 0:n], ALU.mult, r=[tg, tl], w=[ta])
                    for n4 in range(4):
                        w_, twd = wd.next()
                        k.dma(pool, lambda: nc.gpsimd.dma_start(out=w_[:], in_=wdsrc[:, :, n4 * 512:(n4 + 1) * 512]), w=[twd])
                        for s_ in range(SU // 128):
                            ps, tp = py.next()
                            for fc in range(8):
                                MM(ps[:], aT[:, fc, s_ * 128:(s_ + 1) * 128], w_[:, fc, :], fc == 0, False, r=[ta, twd], w=[tp], inc=False)
                            MM(ps[:], onesb[0:1, :], bd[0:1, n4 * 512:(n4 + 1) * 512], False, True, r=[tconst, tbd, ta, twd], w=[tp])
                            y_, ty = yr.next()
                            CP(act if s_ % 2 else dve, y_[:], ps[:], r=[tp], w=[ty])
                            ST(sp, YS[r0 + s_ * 128: r0 + (s_ + 1) * 128, n4 * 512:(n4 + 1) * 512], y_[:], r=[ty])
            k.barrier()
        with ExitStack() as st:
            gt = st.enter_context(_sbt("lg2", [128, D], F32))
            bt = st.enter_context(_sbt("lb2", [128, D], F32))
            tw = Tok()
            LD(act, gt[:], ext[f"ln2_g{li}"].partition_broadcast(128), w=[tw])
            LD(act, bt[:], ext[f"ln2_b{li}"].partition_broadcast(128), w=[tw])
            x1r = Ring(st, "x1m", 2, [128, D], F32)
            ykr = Ring(st, "ykr", 4, [128, D], F32)
            accr = Ring(st, "accr", 2, [128, D], F32)
            xtr = Ring(st, "xtm", 2, [128, NC_, 128], BF16)
            g4r = Ring(st, "g4m", 2, [128, TOPK], F32)
            dir_ = Ring(st, "dim", 2, [128, G, TOPK], I32)
            junk = st.enter_context(_sbt("junk2", [128, D], BF16))
            stat = st.enter_context(_sbt("stat2", [128, 8], F32))
            tj, tstat = Tok(), Tok()
            ptr = PRing([0, 1, 2, 3])
            for ti in range(NT):
                tsl = slice(ti * 128, (ti + 1) * 128)
                x1, tx1 = x1r.next()
                LD(sp, x1[:], X1[tsl, :], w=[tx1])
                g4, tg4 = g4r.next()
                LD(sp, g4[:], GD[tsl, :], w=[tg4])
                di, tdi = dir_.next()
                LD(sp, di[:].rearrange("p g k -> p (g k)"), DI[tsl, :], w=[tdi])
                acc, tacc = accr.next()
                TS(dve, acc[:], x1[:], alpha, None, ALU.mult, None, r=[tx1], w=[tacc])
                for kk_ in range(TOPK):
                    yk, tyk = ykr.next()
                    for g in range(G):
                        k.dma(pool, lambda: nc.gpsimd.indirect_dma_start(out=yk[:, :], out_offset=None, in_=YSp[g][:, :],
                                                                        in_offset=bass.IndirectOffsetOnAxis(ap=di[:, g, kk_:kk_ + 1], axis=0),
                                                                        bounds_check=bnd, oob_is_err=False), r=[tdi], w=[tyk])
                    STT(dve, acc[:], yk[:], g4[:, kk_:kk_ + 1], acc[:], ALU.mult, ALU.add, r=[tyk, tg4, tacc], w=[tacc])
                layer_norm((junk, tj, stat, tstat), acc, tacc, gt, bt, tw, x1, tx1)
                ST(sp, Xout[tsl, :], x1[:], r=[tx1])
                if not last:
                    xt_, txt = xtr.next()
                    transpose_tile(ptr, x1, tx1, xt_, txt)
                    ST(sp, XoutT.rearrange("(c p) t -> p c t", p=128)[:, :, tsl], xt_[:], r=[txt])
            k.barrier()

    kstop = int(os.environ.get("KSTOP", "999"))
    stages = []
    for li in range(L):
        j = li // 2
        Xin = ext["x"] if li == 0 else XA
        last = li == L - 1
        if li % 2 == 0:
            stages.append(lambda j=j: mla_proj(j, XT))
            stages.append(lambda: attention(True))
            wo_ap = ext[f"mla_w_o{j}"]
        else:
            stages.append(lambda j=j: moba_proj(j, XT))
            stages.append(lambda: attention(False))
            wo_ap = ext[f"moba_w_o{j}"]
        stages.append(lambda li=li, wo_ap=wo_ap, Xin=Xin: post_attn(li, wo_ap, Xin))
        stages.append(lambda li=li, last=last: moe(li, XA, XT, last))
    for si, fn in enumerate(stages):
        if si >= kstop:
            break
        fn()
    k.barrier()
    for c in range(0, T, 1024):
        k.dma(sp, lambda: nc.sync.dma_start(out=out[c:c + 1024, :], in_=XA[c:c + 1024, :]))
    k.barrier(only=[sp])
    es.close()
    return nc


def _consts(B, S):
    c = {}
    c["identf"] = np.eye(128, dtype=np.float32)
    c["identb"] = _bf(np.eye(128, dtype=np.float32))
    c["onesb"] = _bf(np.ones((128, 128), np.float32))
    kk = np.arange(128)[:, None, None]
    jj = np.arange(4)[None, :, None]
    qq = np.arange(512)[None, None, :]
    c["cm"] = _bf(np.where(qq >= 128 * jj + kk, 0.0, NEG).astype(np.float32))
    inv = (10000.0 ** (-np.arange(0, 64, 2, dtype=np.float32) / 64)).astype(np.float32)
    c["invf"] = np.concatenate([inv, inv]).reshape(64, 1).astype(np.float32)
    c16 = np.zeros((128, 2, 16), np.float32)
    c16[:, 1, :] = -1e30
    c["c16"] = c16
    c["ustr"] = _bf(np.triu(np.ones((128, 128), np.float32), 1))
    slopes = (2.0 ** (-8.0 * np.arange(1, H + 1, dtype=np.float32) / H)).astype(np.float32)
    s1 = slopes.astype(ml_dtypes.bfloat16).astype(np.float32)
    s2 = (slopes - s1).astype(ml_dtypes.bfloat16).astype(np.float32)
    s3 = (slopes - s1 - s2).astype(ml_dtypes.bfloat16).astype(np.float32)
    sl = np.stack([s1, s2, s3, s1, s2, s3], 1)
    lk = np.zeros((B, H, 28, S), np.float32)
    rq = np.zeros((B, H, 28, S), np.float32)
    blk = np.arange(S) // 256
    lk[:, :, :16, :] = (np.arange(16)[:, None] == blk[None, :]).astype(np.float32)[None, None]
    lk[:, :, 22:28, :] = sl[None, :, :, None]
    rq[:, :, 16:22, :] = sl[None, :, :, None]
    c["lkc"] = _bf(lk)
    c["rqc"] = _bf(rq)
    return c


def cap_of(T, E):
    return (((T * TOPK // E) * 5 // 4) + 255) // 256 * 256


def prepare(inputs, B, S, L, E):
    m = dict(_consts(B, S))
    m["ecap"] = np.ascontiguousarray(np.broadcast_to((np.arange(E) * cap_of(B * S, E)).astype(np.float32)[None, :], (128, E)))
    x = np.asarray(inputs["x"], np.float32).reshape(B * S, D)
    m["x"] = np.ascontiguousarray(x)
    m["xT"] = np.ascontiguousarray(x.T)
    m["pos"] = np.ascontiguousarray(np.asarray(inputs["positions"]).astype(np.int32).reshape(B, S))
    for j in range((L + 1) // 2):
        w_in = np.asarray(inputs["mla_w_in"][j])
        m[f"mla_w_in{j}"] = np.ascontiguousarray(w_in)
        m[f"mla_w_in_sw{j}"] = np.ascontiguousarray(np.concatenate([w_in[:, 1056:1088], w_in[:, 1024:1056]], 1))
        m[f"mla_g_q{j}"] = np.ascontiguousarray(np.asarray(inputs["mla_g_q"][j]).reshape(4, 128).T)
        m[f"mla_g_kv{j}"] = np.ascontiguousarray(np.asarray(inputs["mla_g_kv"][j]).reshape(4, 128).T)
        wq = np.asarray(inputs["mla_w_qb"][j])
        m[f"mla_w_qb{j}"] = np.ascontiguousarray(wq)
        w3 = wq.reshape(512, H, 192)
        m[f"mla_w_qb_sw{j}"] = np.ascontiguousarray(np.concatenate([w3[:, :, 160:192], w3[:, :, 128:160]], 2).reshape(512, H * 64))
        m[f"mla_w_kvb{j}"] = np.ascontiguousarray(np.asarray(inputs["mla_w_kvb"][j]))
        m[f"mla_w_o{j}"] = np.ascontiguousarray(np.asarray(inputs["mla_w_o"][j]))
    for j in range(L // 2):
        m[f"moba_w_qkv{j}"] = np.ascontiguousarray(np.asarray(inputs["moba_w_qkv"][j]))
        m[f"moba_w_o{j}"] = np.ascontiguousarray(np.asarray(inputs["moba_w_o"][j]))
    for li in range(L):
        for nm in ("ln1_g", "ln1_b", "ln2_g", "ln2_b"):
            m[f"{nm}{li}"] = np.ascontiguousarray(np.asarray(inputs[nm][li]).reshape(1, D))
        m[f"moe_w_router{li}"] = np.ascontiguousarray(np.asarray(inputs["moe_w_router"][li]))
        m[f"moe_b_router{li}"] = np.ascontiguousarray(np.asarray(inputs["moe_b_router"][li]).reshape(1, E))
        m[f"moe_b_gu_t{li}"] = np.ascontiguousarray(np.asarray(inputs["moe_b_gu"][li]).reshape(E, 16, 128).transpose(2, 0, 1))
        m[f"moe_b_down{li}"] = np.ascontiguousarray(np.asarray(inputs["moe_b_down"][li]))
        for e in range(E):
            m[f"wgu_{li}_{e}"] = np.ascontiguousarray(np.asarray(inputs["moe_w_gu"][li, e]))
            m[f"wdn_{li}_{e}"] = np.ascontiguousarray(np.asarray(inputs["moe_w_down"][li, e]))
    return m


_NPDT = {np.dtype(np.float32): F32, np.dtype(np.int32): I32, np.dtype(ml_dtypes.bfloat16): BF16}


def run(inputs, B, S, L, E):
    m = prepare(inputs, B, S, L, E)
    names = {n: (a.shape, _NPDT[a.dtype]) for n, a in m.items()}
    nc = build(B, S, L, E, names)
    res = run_bass_kernel_spmd(nc, [m], core_ids=[0])
    return np.asarray(res.results[0]["out"], np.float32).reshape(B, S, D)


def run_multi(inputs, B, S, L, E):
    x = np.asarray(inputs["x"], np.float32).reshape(B, S, D)
    pos = np.asarray(inputs["positions"]).astype(np.int32).reshape(B, S)
    one = dict(inputs)
    one["x"] = x[0:1]
    one["positions"] = pos[0:1]
    base = prepare(one, 1, S, L, E)
    in_maps = []
    for b in range(B):
        m = dict(base)
        m["x"] = np.ascontiguousarray(x[b])
        m["xT"] = np.ascontiguousarray(x[b].T)
        m["pos"] = np.ascontiguousarray(pos[b:b + 1])
        in_maps.append(m)
    names = {n: (a.shape, _NPDT[a.dtype]) for n, a in base.items()}
    nc = build(1, S, L, E, names)
    res = run_bass_kernel_spmd(nc, in_maps, core_ids=list(range(B)))
    return np.stack([np.asarray(r["out"], np.float32).reshape(S, D) for r in res.results], 0)


def kernel(**inputs):
    return run_multi(inputs, 4, 4096, 4, 32)
```

```python
from contextlib import ExitStack
import os
import numpy as np
import ml_dtypes
import concourse.bass as bass
import concourse.mybir as mybir
from concourse.bass_utils import run_bass_kernel_spmd

dt = mybir.dt
F32, BF16, I32 = dt.float32, dt.bfloat16, dt.int32
ALU = mybir.AluOpType
AF = mybir.ActivationFunctionType
AX = mybir.AxisListType

D = 2048
NC_ = 16
H = 16
FF = 1024
TOPK = 4
NEG = -30000.0
LN_EPS = 1e-5
RMS_EPS = 1e-6


class Sem:
    _n = 0

    def __init__(self, h):
        self.h = h
        Sem._n += 1
        self.id = Sem._n
        self.val = 0


class Tok:
    __slots__ = ("w", "r")

    def __init__(self):
        self.w = None
        self.r = {}


class Q:
    EPOCH = 60000

    def __init__(self, K, eng, name, is_pe=False):
        self.K, self.e, self.name, self.is_pe = K, eng, name, is_pe
        self.sem = K.new_sem(name)
        self.retired = []
        self.seen = {}
        self.dma_sems = [None] * K.n_dma_sems
        self.dma_i = 0
        self.pending = False

    def wait(self, ev):
        sem, val = ev
        if self.seen.get(sem.id, 0) >= val:
            return
        self.e.wait_ge(sem.h, val)
        self.seen[sem.id] = val


class K:
    def __init__(self, nc, n_dma_sems=8):
        self.nc = nc
        self.es = ExitStack()
        self.n_dma_sems = n_dma_sems
        self.nsem = 0
        self.pe = Q(self, nc.tensor, "pe", is_pe=True)
        self.act = Q(self, nc.scalar, "act")
        self.dve = Q(self, nc.vector, "dve")
        self.pool = Q(self, nc.gpsimd, "pool")
        self.sp = Q(self, nc.sync, "sp")
        self.qs = [self.pe, self.act, self.dve, self.pool, self.sp]

    def new_sem(self, name):
        self.nsem += 1
        return Sem(self.es.enter_context(self.nc.semaphore(f"s{self.nsem}_{name}")))

    def _deps(self, q, r, w):
        evs = []
        for t in r:
            if t.w is not None:
                evs.append(t.w)
        for t in w:
            if t.w is not None:
                evs.append(t.w)
            evs.extend(t.r.values())
        for ev in evs:
            if q.is_pe and ev[0] is q.sem:
                continue
            q.wait(ev)

    def _record(self, ev, r, w):
        for t in r:
            t.r[ev[0].id] = ev
        for t in w:
            t.w = ev
            t.r = {}

    def op(self, q, fn, r=(), w=(), inc=True):
        self._deps(q, r, w)
        if q.sem.val >= Q.EPOCH and not q.pending:
            q.retired.append(q.sem)
            q.sem = self.new_sem(q.name)
        inst = fn()
        if inc:
            q.sem.val += 1
            inst.then_inc(q.sem.h, 1)
            ev = (q.sem, q.sem.val)
            q.pending = False
        else:
            ev = (q.sem, q.sem.val + 1)
            q.pending = True
        self._record(ev, r, w)
        return ev

    def dma(self, q, fn, r=(), w=()):
        self._deps(q, r, w)
        j = q.dma_i % len(q.dma_sems)
        if q.dma_sems[j] is None:
            q.dma_sems[j] = self.new_sem(f"{q.name}_d{j}")
        s = q.dma_sems[j]
        q.dma_i += 1
        if s.val > 0:
            q.wait((s, s.val))
        if s.val >= Q.EPOCH:
            q.retired.append(s)
            s = q.dma_sems[j] = self.new_sem(f"{q.name}_d{j}")
        inst = fn()
        s.val += 16
        inst.then_inc(s.h, 16)
        ev = (s, s.val)
        self._record(ev, r, w)
        return ev

    def barrier(self, only=None):
        evs = []
        for p in self.qs:
            for s in p.retired + [p.sem] + [d for d in p.dma_sems if d is not None]:
                if s.val > 0:
                    evs.append((s, s.val))
            p.retired = []
        for q in (only or self.qs):
            for ev in evs:
                if q.is_pe and ev[0] is q.sem:
                    continue
                q.wait(ev)


def _bf(a):
    return np.ascontiguousarray(a).astype(ml_dtypes.bfloat16)


def build(B, S, L, E, names):
    nc = bass.Bass("TRN2", target_bir_lowering=False)
    T = B * S
    NT = T // 128
    NB = S // 256
    NQ = S // 512
    ext = {}
    for n, (shp, dty) in names.items():
        ext[n] = nc.dram_tensor(n, list(shp), dty, kind="ExternalInput").ap()
    out = nc.dram_tensor("out", [T, D], F32, kind="ExternalOutput").ap()

    def scratch(name, shape, dtype):
        return nc.dram_tensor(name, list(shape), dtype).ap()

    XA = scratch("XA", [T, D], F32)
    X1 = scratch("X1", [T, D], F32)
    XT = scratch("XT", [D, T], BF16)
    X1T = scratch("X1T", [D, T], BF16)
    OT = scratch("OT", [D, T], BF16)
    QTn = scratch("QTn", [H, 128, T], BF16)
    QTr = scratch("QTr", [H, 64, T], BF16)
    KTn = scratch("KTn", [H, 128, T], BF16)
    KR = scratch("KR", [64, T], BF16)
    VV = scratch("VV", [T, D], BF16)
    CQ = scratch("CQ", [512, T], BF16)
    CKV = scratch("CKV", [512, T], BF16)
    CAP = cap_of(T, E)
    SU = CAP if CAP <= 1024 else CAP // 2
    GD = scratch("GD", [T, TOPK], F32)
    EPP = E
    while EPP * CAP * D * 4 > (256 << 20):
        EPP //= 2
    EPP = min(EPP, int(os.environ.get("KEPP", "64")))
    G = E // EPP
    DI = scratch("DI2", [T, G * TOPK], I32)
    XSp = [scratch(f"XS{g}", [EPP * CAP, D], F32) for g in range(G)]
    YSp = [scratch(f"YS{g}", [EPP * CAP, D], F32) for g in range(G)]
    LK = scratch("LK", [B, H, 28, S], BF16)
    RQ = scratch("RQ", [B, H, 28, S], BF16)

    k = K(nc)
    es = k.es
    _uid = [0]

    def _sbt(name, shape, dtype):
        _uid[0] += 1
        return nc.sbuf_tensor(f"{name}_u{_uid[0]}", shape, dtype)
    pe, act, dve, pool, sp = k.pe, k.act, k.dve, k.pool, k.sp

    def sbp(name, shape, dtype):
        return es.enter_context(_sbt(name, list(shape), dtype))

    PS = [es.enter_context(nc.psum_tensor(f"ps{i}", [128, 512], F32)) for i in range(8)]
    PT = [Tok() for _ in range(8)]
    identf = sbp("identf_s", [128, 128], F32)
    identb = sbp("identb_s", [128, 128], BF16)
    onesb = sbp("onesb_s", [128, 128], BF16)
    cm = sbp("cm_s", [128, 4, 512], BF16)
    c16 = sbp("c16_s", [128, 2, 16], F32)
    ustr = sbp("ustr_s", [128, 128], BF16)
    tconst = Tok()
    k.dma(sp, lambda: nc.sync.dma_start(out=identf[:], in_=ext["identf"]), w=[tconst])
    k.dma(sp, lambda: nc.sync.dma_start(out=identb[:], in_=ext["identb"]), w=[tconst])
    k.dma(sp, lambda: nc.sync.dma_start(out=onesb[:], in_=ext["onesb"]), w=[tconst])
    k.dma(sp, lambda: nc.sync.dma_start(out=cm[:], in_=ext["cm"]), w=[tconst])
    k.dma(sp, lambda: nc.sync.dma_start(out=c16[:], in_=ext["c16"]), w=[tconst])
    k.dma(sp, lambda: nc.sync.dma_start(out=ustr[:], in_=ext["ustr"]), w=[tconst])
    bnd = nc.gpsimd.alloc_register("bnd")
    nc.gpsimd.reg_mov(bnd, EPP * CAP - 1)
    k.barrier()

    def MM(ps, lhsT, rhs, start, stop, r, w, inc=True):
        k.op(pe, lambda: nc.tensor.matmul(ps, lhsT=lhsT, rhs=rhs, start=start, stop=stop), r=r, w=w, inc=inc)

    def TR(ps, in_, r, w):
        k.op(pe, lambda: nc.tensor.transpose(ps, in_, identf[:in_.shape[0], :in_.shape[0]]), r=r, w=w)

    def ACT(out_, in_, func, r, w, **kw):
        k.op(act, lambda: nc.scalar.activation(out=out_, in_=in_, func=func, **kw), r=r, w=w)

    def TT(q, out_, in0, in1, op, r, w):
        k.op(q, lambda: q.e.tensor_tensor(out=out_, in0=in0, in1=in1, op=op), r=r, w=w)

    def TS(q, out_, in0, s1, s2, op0, op1, r, w):
        if op1 is None:
            k.op(q, lambda: q.e.tensor_scalar(out=out_, in0=in0, scalar1=s1, scalar2=None, op0=op0), r=r, w=w)
        else:
            k.op(q, lambda: q.e.tensor_scalar(out=out_, in0=in0, scalar1=s1, scalar2=s2, op0=op0, op1=op1), r=r, w=w)

    def STT(q, out_, in0, scalar, in1, op0, op1, r, w):
        k.op(q, lambda: q.e.scalar_tensor_tensor(out=out_, in0=in0, scalar=scalar, in1=in1, op0=op0, op1=op1), r=r, w=w)

    def CP(q, out_, in_, r, w):
        if q is act:
            k.op(q, lambda: nc.scalar.copy(out=out_, in_=in_), r=r, w=w)
        else:
            k.op(q, lambda: q.e.tensor_copy(out=out_, in_=in_), r=r, w=w)

    def MEMSET(q, ap, val, w):
        k.op(q, lambda: q.e.memset(ap, val), w=w)

    def LD(q, out_, in_, w, r=()):
        k.dma(q, lambda: q.e.dma_start(out=out_, in_=in_), r=r, w=w)

    def ST(q, out_, in_, r):
        k.dma(q, lambda: q.e.dma_start(out=out_, in_=in_), r=r)

    class Ring:
        def __init__(self, st, name, n, shape, dtype):
            self.t = [st.enter_context(_sbt(f"{name}{i}", list(shape), dtype)) for i in range(n)]
            self.k = [Tok() for _ in range(n)]
            self.i = 0

        def next(self):
            j = self.i % len(self.t)
            self.i += 1
            return self.t[j], self.k[j]

    class PRing:
        def __init__(self, idxs):
            self.idxs = idxs
            self.i = 0

        def next(self):
            j = self.idxs[self.i % len(self.idxs)]
            self.i += 1
            return PS[j], PT[j]

    def layer_norm(st_tiles, z, tz, gt, bt, tgb, o, to):
        junk, tj, stat, tstat = st_tiles
        ACT(junk[:], z[:], AF.Copy, r=[tz], w=[tj, tstat], accum_out=stat[:, 0:1])
        ACT(junk[:], z[:], AF.Square, r=[tz], w=[tj, tstat], accum_out=stat[:, 1:2])
        TS(dve, stat[:, 2:3], stat[:, 0:1], 1.0 / D, None, ALU.mult, None, r=[tstat], w=[tstat])
        TT(dve, stat[:, 3:4], stat[:, 2:3], stat[:, 2:3], ALU.mult, r=[tstat], w=[tstat])
        STT(dve, stat[:, 4:5], stat[:, 1:2], 1.0 / D, stat[:, 3:4], ALU.mult, ALU.subtract, r=[tstat], w=[tstat])
        TS(dve, stat[:, 4:5], stat[:, 4:5], LN_EPS, None, ALU.add, None, r=[tstat], w=[tstat])
        ACT(stat[:, 5:6], stat[:, 4:5], AF.Sqrt, r=[tstat], w=[tstat])
        k.op(dve, lambda: nc.vector.reciprocal(out=stat[:, 6:7], in_=stat[:, 5:6]), r=[tstat], w=[tstat])
        TS(dve, o[:], z[:], stat[:, 2:3], stat[:, 6:7], ALU.subtract, ALU.mult, r=[tz, tstat], w=[to])
        TT(dve, o[:], o[:], gt[:], ALU.mult, r=[to, tgb], w=[to])
        TT(dve, o[:], o[:], bt[:], ALU.add, r=[to, tgb], w=[to])

    def transpose_tile(pr, x, tx, xt, txt):
        for g in range(4):
            ps, tp = pr.next()
            for j in range(4):
                c = g * 4 + j
                TR(ps[:, j * 128:(j + 1) * 128], x[:, c * 128:(c + 1) * 128], r=[tx, tconst], w=[tp])
            CP(act if g % 2 else dve, xt[:, g * 4:(g + 1) * 4, :],
               ps[:].rearrange("p (j t) -> p j t", j=4), r=[tp], w=[txt])

    for c in range(0, D, 512):
        k.dma(pool, lambda: nc.gpsimd.dma_start(out=XT[c:c + 512, :], in_=ext["xT"][c:c + 512, :]))
    with ExitStack() as st:
        COS = scratch("COS", [B, 64, S], F32)
        SIN = scratch("SIN", [B, 64, S], F32)
        pi_ = st.enter_context(_sbt("pos_i", [64, S], I32))
        pf = st.enter_context(_sbt("pos_f", [64, S], F32))
        a1 = st.enter_context(_sbt("ang1", [64, S], F32))
        a2 = st.enter_context(_sbt("ang2", [64, S], F32))
        invf = st.enter_context(_sbt("invf_s", [64, 1], F32))
        rb = st.enter_context(_sbt("rb", [3, S], F32))
        ra = st.enter_context(_sbt("ra", [3, S], F32))
        rbb = st.enter_context(_sbt("rbb", [3, 2, S], BF16))
        rab = st.enter_context(_sbt("rab", [3, 2, S], BF16))
        tq_ = st.enter_context(_sbt("tq_", [64, S], F32))
        ri = st.enter_context(_sbt("ri", [3, S], I32))
        tp_, tf_, t1_, t2_, tv_, tr_, ttq = Tok(), Tok(), Tok(), Tok(), Tok(), Tok(), Tok()
        LD(sp, invf[:], ext["invf"], w=[tv_])
        k.dma(pool, lambda: nc.gpsimd.dma_start(out=LK.rearrange("b h r s -> (b h r) s"), in_=ext["lkc"].rearrange("b h r s -> (b h r) s")))
        k.dma(pool, lambda: nc.gpsimd.dma_start(out=RQ.rearrange("b h r s -> (b h r) s"), in_=ext["rqc"].rearrange("b h r s -> (b h r) s")))
        k.barrier()
        for b in range(B):
            LD(sp, pi_[:], ext["pos"][b:b + 1, :].partition_broadcast(64), w=[tp_])
            CP(dve, pf[:], pi_[:], r=[tp_], w=[tf_])
            TS(dve, a1[:], pf[:], invf[:, 0:1], None, ALU.mult, None, r=[tf_, tv_], w=[t1_])
            TS(dve, a2[:], a1[:], float(np.pi / 2), None, ALU.add, None, r=[t1_], w=[t2_])
            TWO_PI = float(2 * np.pi)
            for a, ta in ((a1, t1_), (a2, t2_)):
                TS(dve, tq_[:], a[:], 1.0 / TWO_PI, None, ALU.mult, None, r=[ta], w=[ttq])
                CP(dve, pi_[:], tq_[:], r=[ttq, tp_], w=[tp_])
                CP(dve, tq_[:], pi_[:], r=[tp_], w=[ttq])
                STT(dve, a[:], tq_[:], -TWO_PI, a[:], ALU.mult, ALU.add, r=[ttq, ta], w=[ta])
                TS(dve, tq_[:], a[:], float(np.pi), TWO_PI, ALU.is_gt, ALU.mult, r=[ta], w=[ttq])
                TT(dve, a[:], a[:], tq_[:], ALU.subtract, r=[ta, ttq], w=[ta])
                TS(dve, tq_[:], a[:], float(-np.pi), TWO_PI, ALU.is_lt, ALU.mult, r=[ta], w=[ttq])
                TT(dve, a[:], a[:], tq_[:], ALU.add, r=[ta, ttq], w=[ta])
                TS(dve, a[:], a[:], float(np.pi), float(-np.pi), ALU.min, ALU.max, r=[ta], w=[ta])
                ACT(a[:], a[:], AF.Sin, r=[ta], w=[ta])
            TS(dve, a1[0:32, :], a1[0:32, :], -1.0, None, ALU.mult, None, r=[t1_], w=[t1_])
            ST(sp, COS[b], a2[:], r=[t2_])
            ST(sp, SIN[b], a1[:], r=[t1_])
            TS(dve, rb[:], pf[0:3, :], pf[0:3, 0:1], None, ALU.subtract, None, r=[tf_], w=[tr_])
            TS(dve, ra[:], rb[:], 1.0 / 64, None, ALU.mult, None, r=[tr_], w=[tr_])
            CP(dve, ri[:], ra[:], r=[tr_], w=[tr_])
            CP(dve, ra[:], ri[:], r=[tr_], w=[tr_])
            TS(dve, ra[:], ra[:], 64.0, None, ALU.mult, None, r=[tr_], w=[tr_])
            TT(dve, rb[:], rb[:], ra[:], ALU.subtract, r=[tr_], w=[tr_])
            CP(dve, rab[:, 0, :], ra[:], r=[tr_], w=[tr_])
            CP(dve, rbb[:, 0, :], rb[:], r=[tr_], w=[tr_])
            TS(dve, rab[:, 1, :], ra[:], -1.0, None, ALU.mult, None, r=[tr_], w=[tr_])
            TS(dve, rbb[:, 1, :], rb[:], -1.0, None, ALU.mult, None, r=[tr_], w=[tr_])
            for h in range(H):
                ST(sp, LK[b, h, 16:19, :], rab[:, 0, :], r=[tr_])
                ST(sp, LK[b, h, 19:22, :], rbb[:, 0, :], r=[tr_])
                ST(sp, RQ[b, h, 22:25, :], rab[:, 1, :], r=[tr_])
                ST(sp, RQ[b, h, 25:28, :], rbb[:, 1, :], r=[tr_])
        k.barrier()

    def mla_proj(j, Xin_T):
        sc = float((128 + 64) ** -0.5)
        with ExitStack() as st:
            win = st.enter_context(_sbt("win", [128, NC_, 1088], BF16))
            wins = st.enter_context(_sbt("wins", [128, NC_, 64], BF16))
            gq = st.enter_context(_sbt("gq", [128, 4], F32))
            gkv = st.enter_context(_sbt("gkv", [128, 4], F32))
            tw = Tok()
            k.dma(pool, lambda: nc.gpsimd.dma_start(out=win[:], in_=ext[f"mla_w_in{j}"].rearrange("(c p) n -> p c n", p=128)), w=[tw])
            k.dma(pool, lambda: nc.gpsimd.dma_start(out=wins[:], in_=ext[f"mla_w_in_sw{j}"].rearrange("(c p) n -> p c n", p=128)), w=[tw])
            LD(sp, gq[:], ext[f"mla_g_q{j}"], w=[tw])
            LD(sp, gkv[:], ext[f"mla_g_kv{j}"], w=[tw])
            xr = Ring(st, "xr", 2, [128, NC_, 512], BF16)
            cf = Ring(st, "cf", 2, [128, 4, 512], F32)
            sq = Ring(st, "sq", 2, [128, 512], BF16)
            rs = Ring(st, "rs", 2, [128, 512], F32)
            cn = Ring(st, "cn", 2, [128, 4, 512], BF16)
            cs = Ring(st, "cs", 2, [64, 512], F32)
            t1r = Ring(st, "t1r", 2, [64, 512], F32)
            kro = Ring(st, "kro", 2, [64, 512], BF16)
            pa = PRing([0, 1, 2, 3])
            pss = PRing([4, 5])
            pk = PRing([6, 7])
            for tt in range(T // 512):
                b = (tt * 512) // S
                s0 = tt * 512 - b * S
                x_, tx = xr.next()
                LD(sp, x_[:], Xin_T.rearrange("(c p) t -> p c t", p=128)[:, :, tt * 512:(tt + 1) * 512], w=[tx])
                cos_, tcs = cs.next()
                sin_, tsn = cs.next()
                LD(sp, cos_[:], COS[b, :, s0:s0 + 512], w=[tcs])
                LD(sp, sin_[:], SIN[b, :, s0:s0 + 512], w=[tsn])
                for which, goff, gvec, dst in ((0, 0, gq, CQ), (1, 512, gkv, CKV)):
                    c_, tc = cf.next()
                    ss, tss = pss.next()
                    for ch in range(4):
                        ps, tp = pa.next()
                        for c in range(NC_):
                            MM(ps[:], win[:, c, goff + ch * 128: goff + (ch + 1) * 128], x_[:, c, :], c == 0, c == NC_ - 1, r=[tw, tx], w=[tp])
                        CP(act, c_[:, ch, :], ps[:], r=[tp], w=[tc])
                        s_, ts_ = sq.next()
                        ACT(s_[:], ps[:], AF.Square, r=[tp], w=[ts_])
                        MM(ss[:], onesb[:], s_[:], ch == 0, ch == 3, r=[ts_, tconst], w=[tss])
                    r_, trs = rs.next()
                    TS(dve, r_[:], ss[:], 1.0 / 512, RMS_EPS, ALU.mult, ALU.add, r=[tss], w=[trs])
                    ACT(r_[:], r_[:], AF.Sqrt, r=[trs], w=[trs])
                    k.op(dve, lambda: nc.vector.reciprocal(out=r_[:], in_=r_[:]), r=[trs], w=[trs])
                    n_, tn = cn.next()
                    for ch in range(4):
                        STT(dve, n_[:, ch, :], c_[:, ch, :], gvec[:, ch:ch + 1], r_[:], ALU.mult, ALU.mult, r=[tc, trs, tw], w=[tn])
                    ST(sp, dst.rearrange("(c p) t -> p c t", p=128)[:, :, tt * 512:(tt + 1) * 512], n_[:], r=[tn])
                p1, tp1 = pk.next()
                p2, tp2 = pk.next()
                for c in range(NC_):
                    MM(p1[0:64, :], win[:, c, 1024:1088], x_[:, c, :], c == 0, c == NC_ - 1, r=[tw, tx], w=[tp1])
                for c in range(NC_):
                    MM(p2[0:64, :], wins[:, c, :], x_[:, c, :], c == 0, c == NC_ - 1, r=[tw, tx], w=[tp2])
                u1, tu1 = t1r.next()
                u2, tu2 = t1r.next()
                TT(dve, u1[:], p1[0:64, :], cos_[:], ALU.mult, r=[tp1, tcs], w=[tu1])
                TT(dve, u2[:], p2[0:64, :], sin_[:], ALU.mult, r=[tp2, tsn], w=[tu2])
                ko, tko = kro.next()
                TT(dve, ko[:], u1[:], u2[:], ALU.add, r=[tu1, tu2], w=[tko])
                ST(sp, KR[:, tt * 512:(tt + 1) * 512], ko[:], r=[tko])
            k.barrier()
        with ExitStack() as st:
            wq = st.enter_context(_sbt("wq", [128, 4, H * 192], BF16))
            wqs = st.enter_context(_sbt("wqs", [128, 4, H * 64], BF16))
            wkv = st.enter_context(_sbt("wkv", [128, 4, H * 256], BF16))
            tw = Tok()
            k.dma(pool, lambda: nc.gpsimd.dma_start(out=wq[:], in_=ext[f"mla_w_qb{j}"].rearrange("(c p) n -> p c n", p=128)), w=[tw])
            k.dma(pool, lambda: nc.gpsimd.dma_start(out=wqs[:], in_=ext[f"mla_w_qb_sw{j}"].rearrange("(c p) n -> p c n", p=128)), w=[tw])
            k.dma(pool, lambda: nc.gpsimd.dma_start(out=wkv[:], in_=ext[f"mla_w_kvb{j}"].rearrange("(c p) n -> p c n", p=128)), w=[tw])
            cqr = Ring(st, "cqr", 2, [128, 4, 512], BF16)
            ckr = Ring(st, "ckr", 2, [128, 4, 512], BF16)
            cs = Ring(st, "cs2", 4, [64, 512], F32)
            qn = Ring(st, "qn", 3, [128, 512], BF16)
            qr = Ring(st, "qr", 3, [64, 512], BF16)
            kn = Ring(st, "kn", 3, [128, 512], BF16)
            vt = Ring(st, "vt", 2, [128, D], BF16)
            u1r = Ring(st, "u1r", 3, [64, 512], F32)
            u2r = Ring(st, "u2r", 3, [64, 512], F32)
            pa = PRing([0, 1, 2])
            pb = PRing([3, 4])
            pc = PRing([5, 6, 7])
            for tt in range(T // 512):
                b = (tt * 512) // S
                s0 = tt * 512 - b * S
                cq_, tcq = cqr.next()
                ck_, tck = ckr.next()
                LD(sp, cq_[:], CQ.rearrange("(c p) t -> p c t", p=128)[:, :, tt * 512:(tt + 1) * 512], w=[tcq])
                LD(sp, ck_[:], CKV.rearrange("(c p) t -> p c t", p=128)[:, :, tt * 512:(tt + 1) * 512], w=[tck])
                cos_, tcs = cs.next()
                sin_, tsn = cs.next()
                LD(sp, cos_[:], COS[b, :, s0:s0 + 512], w=[tcs])
                LD(sp, sin_[:], SIN[b, :, s0:s0 + 512], w=[tsn])
                for h in range(H):
                    qn_, tqn = qn.next()
                    qr_, tqr = qr.next()
                    kn_, tkn = kn.next()
                    ps, tp = pa.next()
                    for c in range(4):
                        MM(ps[:], wq[:, c, h * 192:h * 192 + 128], cq_[:, c, :], c == 0, c == 3, r=[tw, tcq], w=[tp])
                    ACT(qn_[:], ps[:], AF.Copy, r=[tp], w=[tqn], scale=sc)
                    ST(sp, QTn[h, :, tt * 512:(tt + 1) * 512], qn_[:], r=[tqn])
                    p1, tp1 = pb.next()
                    p2, tp2 = pb.next()
                    for c in range(4):
                        MM(p1[0:64, :], wq[:, c, h * 192 + 128:h * 192 + 192], cq_[:, c, :], c == 0, c == 3, r=[tw, tcq], w=[tp1])
                    for c in range(4):
                        MM(p2[0:64, :], wqs[:, c, h * 64:(h + 1) * 64], cq_[:, c, :], c == 0, c == 3, r=[tw, tcq], w=[tp2])
                    u1, tu1 = u1r.next()
                    u2, tu2 = u2r.next()
                    STT(dve, u1[:], p1[0:64, :], sc, cos_[:], ALU.mult, ALU.mult, r=[tp1, tcs], w=[tu1])
                    STT(dve, u2[:], p2[0:64, :], sc, sin_[:], ALU.mult, ALU.mult, r=[tp2, tsn], w=[tu2])
                    TT(pool, qr_[:], u1[:], u2[:], ALU.add, r=[tu1, tu2], w=[tqr])
                    ST(sp, QTr[h, :, tt * 512:(tt + 1) * 512], qr_[:], r=[tqr])
                    ps, tp = pc.next()
                    for c in range(4):
                        MM(ps[:], wkv[:, c, h * 256:h * 256 + 128], ck_[:, c, :], c == 0, c == 3, r=[tw, tck], w=[tp])
                    CP(act, kn_[:], ps[:], r=[tp], w=[tkn])
                    ST(sp, KTn[h, :, tt * 512:(tt + 1) * 512], kn_[:], r=[tkn])
                for sub in range(4):
                    v_, tv = vt.next()
                    for hg in range(4):
                        ps, tp = pc.next()
                        for c in range(4):
                            MM(ps[:].rearrange("p (h d) -> p h d", h=4), ck_[:, c, sub * 128:(sub + 1) * 128],
                               wkv[:, c, :].rearrange("p (h j) -> p h j", j=256)[:, hg * 4:(hg + 1) * 4, 128:256],
                               c == 0, c == 3, r=[tw, tck], w=[tp])
                        CP(dve if hg % 2 else act, v_[:, hg * 512:(hg + 1) * 512], ps[:], r=[tp], w=[tv])
                    ST(sp, VV[tt * 512 + sub * 128: tt * 512 + (sub + 1) * 128, :], v_[:], r=[tv])
            k.barrier()

    def moba_proj(j, Xin_T):
        sc = float(128 ** -0.5)
        TH = min(T, 2048)
        wsrc = ext[f"moba_w_qkv{j}"].rearrange("(c p) n -> p c n", p=128)
        with ExitStack() as st:
            xs = st.enter_context(_sbt("xs", [128, NC_, TH], BF16))
            km = st.enter_context(_sbt("km", [128, B, H, NB], F32))
            kmb = st.enter_context(_sbt("kmb", [128, B, H, NB], BF16))
            tkm = Tok()
            tkmb = Tok()
            txs = Tok()
            wr = Ring(st, "wr", 3, [128, NC_, 512], BF16)
            ob = Ring(st, "ob", 3, [128, 512], BF16)
            vt = Ring(st, "vt", 2, [128, 512], BF16)
            gmr = Ring(st, "gmr", 3, [128, 16], F32)
            m8r = Ring(st, "m8r", 3, [128, 8], F32)
            mbr = Ring(st, "mbr", 3, [128, 16], F32)
            mtr = Ring(st, "mtr", 3, [16, 512], BF16)
            pa = PRing([0, 1, 2, 3])
            pg = PRing([4, 5])
            pt_ = PRing([6, 7])
            for th in range(T // TH):
                t0 = th * TH
                kmb_done = [False]
                for q4 in range(TH // 512):
                    LD(sp, xs[:, :, q4 * 512:(q4 + 1) * 512],
                       Xin_T.rearrange("(c p) t -> p c t", p=128)[:, :, t0 + q4 * 512: t0 + (q4 + 1) * 512], w=[txs])
                for slab in ([int(v) for v in os.environ['KSLABS'].split(',')] if os.environ.get('KSLABS') else [4, 5, 6, 7, 8, 9, 10, 11, 0, 1, 2, 3]):
                    w_, tw = wr.next()
                    k.dma(pool, lambda: nc.gpsimd.dma_start(out=w_[:], in_=wsrc[:, :, slab * 512:(slab + 1) * 512]), w=[tw])
                    kind = slab // 4
                    if kind == 0 and not kmb_done[0]:
                        CP(dve, kmb[:].rearrange("p b h m -> p (b h m)"), km[:].rearrange("p b h m -> p (b h m)"), r=[tkm], w=[tkmb])
                        kmb_done[0] = True
                    if kind == 2:
                        for sub in range(TH // 128):
                            ps, tp = pa.next()
                            for c in range(NC_):
                                MM(ps[:], xs[:, c, sub * 128:(sub + 1) * 128], w_[:, c, :], c == 0, c == NC_ - 1, r=[txs, tw], w=[tp])
                            v_, tv = vt.next()
                            CP(act if sub % 2 else dve, v_[:], ps[:], r=[tp], w=[tv])
                            ST(sp, VV[t0 + sub * 128: t0 + (sub + 1) * 128, (slab - 8) * 512:(slab - 7) * 512], v_[:], r=[tv])
                        continue
                    KM = int(os.environ.get("KMODE", "9"))
                    for hh in range(4 if KM > 0 else 0):
                        h = (slab % 4) * 4 + hh
                        for tq in range(TH // 512):
                            tg = t0 + tq * 512
                            b = tg // S
                            s0 = tg - b * S
                            ps, tp = pa.next()
                            for c in range(NC_):
                                MM(ps[:], w_[:, c, hh * 128:(hh + 1) * 128], xs[:, c, tq * 512:(tq + 1) * 512], c == 0, c == NC_ - 1, r=[txs, tw], w=[tp])
                            o_, to = ob.next()
                            if kind == 1:
                                blk0 = s0 // 256
                                ACT(o_[:, 0:256], ps[:, 0:256], AF.Copy, r=[tp], w=[to, tkm], accum_out=km[:, b, h, blk0:blk0 + 1])
                                ACT(o_[:, 256:512], ps[:, 256:512], AF.Copy, r=[tp], w=[to, tkm], accum_out=km[:, b, h, blk0 + 1:blk0 + 2])
                                ST(sp, KTn[h, :, tg:tg + 512], o_[:], r=[to])
                            else:
                                ACT(o_[:], ps[:], AF.Copy, r=[tp], w=[to], scale=sc)
                                ST(sp, QTn[h, :, tg:tg + 512], o_[:], r=[to])
                                mt, tmt = mtr.next()
                                for qs in range(4 if os.environ.get("KNOGATE") is None else 0):
                                    qb = (s0 + qs * 128) // 256
                                    mb, tmb = mbr.next()
                                    CP(dve, mb[:], c16[:, 0, :], r=[tconst], w=[tmb])
                                    if qb > 0:
                                        pgs, tpg = pg.next()
                                        MM(pgs[:, 0:NB], o_[:, qs * 128:(qs + 1) * 128], kmb[:, b, h, :], True, True, r=[to, tkmb], w=[tpg])
                                        gm, tgm = gmr.next()
                                        CP(dve, gm[:], c16[:, 1, :], r=[tconst], w=[tgm])
                                        CP(dve, gm[:, 0:qb], pgs[:, 0:qb], r=[tpg], w=[tgm])
                                        m8, tm8 = m8r.next()
                                        k.op(dve, lambda: nc.vector.max(out=m8[:], in_=gm[:]), r=[tgm], w=[tm8])
                                        TS(dve, mb[:, 0:qb], gm[:, 0:qb], m8[:, 2:3], NEG, ALU.is_lt, ALU.mult, r=[tgm, tm8], w=[tmb])
                                    ptp, tpt = pt_.next()
                                    TR(ptp[0:16, 0:128], mb[:], r=[tmb, tconst], w=[tpt])
                                    CP(act, mt[:, qs * 128:(qs + 1) * 128], ptp[0:16, 0:128], r=[tpt], w=[tmt])
                                ST(sp, RQ[b, h, 0:16, s0:s0 + 512], mt[:], r=[tmt])
            k.barrier()

    def attention(is_mla):
        with ExitStack() as st:
            kt = Ring(st, "kt", 2, [128, S], BF16)
            qt = Ring(st, "qt", 2, [128, S], BF16)
            vv = Ring(st, "vv", 2, [128, S // 128, 128], BF16)
            if is_mla:
                krs = st.enter_context(_sbt("krs", [64, S], BF16))
                tkr = Tok()
                qrr = Ring(st, "qrr", 2, [64, S], BF16)
            else:
                lkr = Ring(st, "lkr", 2, [28, S], BF16)
                rqr = Ring(st, "rqr", 2, [28, S], BF16)
            pr = Ring(st, "pr", 3, [128, 512], BF16)
            rl = Ring(st, "rl", 2, [128, 512], F32)
            on = Ring(st, "on", 2, [128, 512], BF16)
            psS = PRing([0, 1, 2])
            psO = PRing([3, 4])
            psL = PRing([5, 6])
            for b in range(B):
                if is_mla:
                    LD(sp, krs[:], KR[:, b * S:(b + 1) * S], w=[tkr])
                for h in range(H):
                    k_, tk = kt.next()
                    q_, tq = qt.next()
                    v_, tv = vv.next()
                    LD(sp, k_[:], KTn[h, :, b * S:(b + 1) * S], w=[tk])
                    LD(sp, q_[:], QTn[h, :, b * S:(b + 1) * S], w=[tq])
                    LD(sp, v_[:], VV[b * S:(b + 1) * S, h * 128:(h + 1) * 128].rearrange("(t p) d -> p t d", p=128), w=[tv])
                    if is_mla:
                        q2, tq2 = qrr.next()
                        LD(sp, q2[:], QTr[h, :, b * S:(b + 1) * S], w=[tq2])
                    else:
                        lk, tlk = lkr.next()
                        rq, trq = rqr.next()
                        LD(sp, lk[:], LK[b, h], w=[tlk])
                        LD(sp, rq[:], RQ[b, h], w=[trq])
                    for qi in range(NQ):
                        po, tpo = psO.next()
                        pl, tpl = psL.next()
                        nk = 4 * (qi + 1)
                        qsl = slice(qi * 512, (qi + 1) * 512)
                        for ki in range(nk):
                            ksl = slice(ki * 128, (ki + 1) * 128)
                            diag = ki >= 4 * qi
                            ps, tps = psS.next()
                            MM(ps[:], k_[:, ksl], q_[:, qsl], True, False, r=[tk, tq], w=[tps])
                            if is_mla:
                                MM(ps[:], krs[:, ksl], q2[:, qsl], False, not diag, r=[tkr, tq2], w=[tps])
                            else:
                                MM(ps[:], lk[:, ksl], rq[:, qsl], False, not diag, r=[tlk, trq], w=[tps])
                            if diag:
                                MM(ps[:], identb[:], cm[:, ki - 4 * qi, :], False, True, r=[tconst], w=[tps])
                            p_, tp = pr.next()
                            ACT(p_[:], ps[:], AF.Exp, r=[tps], w=[tp])
                            MM(po[:], v_[:, ki, :], p_[:], ki == 0, ki == nk - 1, r=[tv, tp], w=[tpo])
                            MM(pl[:], onesb[:], p_[:], ki == 0, ki == nk - 1, r=[tconst, tp], w=[tpl])
                        r_, tr = rl.next()
                        k.op(dve, lambda: nc.vector.reciprocal(out=r_[:], in_=pl[:]), r=[tpl], w=[tr])
                        o_, to = on.next()
                        TT(dve, o_[:], po[:], r_[:], ALU.mult, r=[tpo, tr], w=[to])
                        ST(sp, OT[h * 128:(h + 1) * 128, b * S + qi * 512: b * S + (qi + 1) * 512], o_[:], r=[to])
            k.barrier()

    def post_attn(li, wo_ap, Xin):
        alpha = float((2.0 * L) ** 0.25)
        with ExitStack() as st:
            wo = st.enter_context(_sbt("wo", [128, NC_, D], BF16))
            wrt = st.enter_context(_sbt("wrt", [128, NC_, E], BF16))
            brt = st.enter_context(_sbt("brt", [1, E], BF16))
            gt = st.enter_context(_sbt("lg", [128, D], F32))
            bt = st.enter_context(_sbt("lb", [128, D], F32))
            tw = Tok()
            for c4 in range(4):
                k.dma(pool, lambda: nc.gpsimd.dma_start(out=wo[:, c4 * 4:(c4 + 1) * 4, :], in_=wo_ap.rearrange("(c p) n -> p c n", p=128)[:, c4 * 4:(c4 + 1) * 4, :]), w=[tw])
            k.dma(pool, lambda: nc.gpsimd.dma_start(out=wrt[:], in_=ext[f"moe_w_router{li}"].rearrange("(c p) n -> p c n", p=128)), w=[tw])
            k.dma(pool, lambda: nc.gpsimd.dma_start(out=brt[:], in_=ext[f"moe_b_router{li}"]), w=[tw])
            LD(act, gt[:], ext[f"ln1_g{li}"].partition_broadcast(128), w=[tw])
            LD(act, bt[:], ext[f"ln1_b{li}"].partition_broadcast(128), w=[tw])
            otr = Ring(st, "otr", 2, [128, NC_, 128], BF16)
            xr = Ring(st, "xr", 2, [128, D], F32)
            zr = Ring(st, "zr", 2, [128, D], F32)
            x1r = Ring(st, "x1r", 2, [128, D], F32)
            xtr = Ring(st, "xtr", 2, [128, NC_, 128], BF16)
            junk = st.enter_context(_sbt("junk", [128, D], BF16))
            stat = st.enter_context(_sbt("stat", [128, 8], F32))
            tj, tstat = Tok(), Tok()
            lgr = Ring(st, "lgr", 2, [128, E], F32)
            exr = Ring(st, "exr", 2, [128, E], F32)
            mkr = Ring(st, "mkr", 2, [128, E], BF16)
            g4r = Ring(st, "g4r", 3, [128, TOPK], F32)
            dsr = Ring(st, "dsr", 2, [128, E], F32)
            d4r = Ring(st, "d4r", 2, [128, TOPK], F32)
            dir_ = Ring(st, "dir", 3, [128, G, TOPK], I32)
            dgr = Ring(st, "dgr", 2, [128, 2, TOPK], F32)
            basecap = st.enter_context(_sbt("basecap", [128, E], F32))
            tbase = Tok()
            LD(sp, basecap[:], ext["ecap"], w=[tbase])
            m8r = Ring(st, "m8r", 2, [128, 8], F32)
            smr = Ring(st, "smr", 2, [128, 4], F32)
            py = PRing([0, 1, 2, 3])
            ptr = PRing([4, 5])
            pl = PRing([6, 7])
            for ti in range(NT):
                tsl = slice(ti * 128, (ti + 1) * 128)
                o_, to = otr.next()
                LD(sp, o_[:], OT.rearrange("(c p) t -> p c t", p=128)[:, :, tsl], w=[to])
                x_, tx = xr.next()
                LD(sp, x_[:], Xin[tsl, :], w=[tx])
                z_, tz = zr.next()
                for n4 in range(4):
                    ps, tp = py.next()
                    for c in range(NC_):
                        MM(ps[:], o_[:, c, :], wo[:, c, n4 * 512:(n4 + 1) * 512], c == 0, c == NC_ - 1, r=[to, tw], w=[tp])
                    STT(dve, z_[:, n4 * 512:(n4 + 1) * 512], x_[:, n4 * 512:(n4 + 1) * 512], alpha, ps[:], ALU.mult, ALU.add, r=[tx, tp], w=[tz])
                x1, tx1 = x1r.next()
                layer_norm((junk, tj, stat, tstat), z_, tz, gt, bt, tw, x1, tx1)
                ST(sp, X1[tsl, :], x1[:], r=[tx1])
                xt_, txt = xtr.next()
                transpose_tile(ptr, x1, tx1, xt_, txt)
                ST(sp, X1T.rearrange("(c p) t -> p c t", p=128)[:, :, tsl], xt_[:], r=[txt])
                ps, tp = pl.next()
                for c in range(NC_):
                    MM(ps[:, 0:E], xt_[:, c, :], wrt[:, c, :], c == 0, False, r=[txt, tw], w=[tp])
                MM(ps[:, 0:E], onesb[0:1, :], brt[:], False, True, r=[tconst, tw], w=[tp])
                lg, tlg = lgr.next()
                CP(dve, lg[:], ps[:, 0:E], r=[tp], w=[tlg])
                m8, tm8 = m8r.next()
                k.op(dve, lambda: nc.vector.max(out=m8[:], in_=lg[:]), r=[tlg], w=[tm8])
                sm, tsm = smr.next()
                TS(dve, sm[:, 0:1], m8[:, 0:1], -1.0, None, ALU.mult, None, r=[tm8], w=[tsm])
                g4, tg4 = g4r.next()
                ACT(g4[:], m8[:, 0:TOPK], AF.Exp, r=[tm8, tsm], w=[tg4], bias=sm[:, 0:1], scale=1.0)
                k.op(dve, lambda: nc.vector.tensor_reduce(out=sm[:, 1:2], in_=g4[:], axis=AX.X, op=ALU.add), r=[tg4], w=[tsm])
                k.op(dve, lambda: nc.vector.reciprocal(out=sm[:, 2:3], in_=sm[:, 1:2]), r=[tsm], w=[tsm])
                TS(dve, g4[:], g4[:], sm[:, 2:3], None, ALU.mult, None, r=[tg4, tsm], w=[tg4])
                ST(sp, GD[tsl, :], g4[:], r=[tg4])
                mk, tmk = mkr.next()
                TS(dve, mk[:], lg[:], m8[:, TOPK - 1:TOPK], None, ALU.is_ge, None, r=[tlg, tm8], w=[tmk])
                pp, tpp = pl.next()
                MM(pp[:, 0:E], ustr[:], mk[:], True, True, r=[tconst, tmk], w=[tpp])
                pc, tpc = pl.next()
                MM(pc[:, 0:E], onesb[:], mk[:], True, True, r=[tconst, tmk], w=[tpc])
                ds, tds = dsr.next()
                TT(dve, ds[:], pp[:, 0:E], basecap[:], ALU.add, r=[tpp, tbase], w=[tds])
                TT(dve, basecap[:], basecap[:], pc[:, 0:E], ALU.add, r=[tpc, tbase], w=[tbase])
                d4, td4 = d4r.next()
                ex, tex = exr.next()
                for kk_ in range(TOPK):
                    k.op(dve, lambda: nc.vector.scalar_tensor_tensor(out=ex[:], in0=lg[:], scalar=m8[:, kk_:kk_ + 1], in1=ds[:],
                                                                     op0=ALU.is_equal, op1=ALU.mult, accum_out=d4[:, kk_:kk_ + 1]),
                         r=[tlg, tm8, tds], w=[tex, td4])
                di, tdi = dir_.next()
                for g in range(G):
                    if G == 1:
                        CP(dve, di[:, 0, :], d4[:], r=[td4], w=[tdi])
                    else:
                        dg, tdg = dgr.next()
                        TS(dve, dg[:, 0, :], d4[:], float(g * EPP * CAP), None, ALU.subtract, None, r=[td4], w=[tdg])
                        TS(dve, dg[:, 1, :], dg[:, 0, :], 0.0, 1.0e9, ALU.is_lt, ALU.mult, r=[tdg], w=[tdg])
                        TT(dve, dg[:, 0, :], dg[:, 0, :], dg[:, 1, :], ALU.add, r=[tdg], w=[tdg])
                        CP(dve, di[:, g, :], dg[:, 0, :], r=[tdg], w=[tdi])
                ST(sp, DI[tsl, :], di[:].rearrange("p g k -> p (g k)"), r=[tdi])
                for g in range(G):
                    for kk_ in range(TOPK):
                        k.dma(pool, lambda: nc.gpsimd.indirect_dma_start(out=XSp[g][:, :], out_offset=bass.IndirectOffsetOnAxis(ap=di[:, g, kk_:kk_ + 1], axis=0),
                                                                        in_=x1[:, :], in_offset=None, bounds_check=bnd, oob_is_err=False),
                              r=[tx1, tdi])
            k.barrier()

    def moe(li, Xout, XoutT, last):
        alpha = float((2.0 * L) ** 0.25)
        NTS = [(o, min(512, SU - o)) for o in range(0, SU, 512)]
        with ExitStack() as st:
            bgu = st.enter_context(_sbt("bgu", [128, E, 16], F32))
            tw = Tok()
            LD(sp, bgu[:], ext[f"moe_b_gu_t{li}"], w=[tw])
            xsT = st.enter_context(_sbt("xsT", [128, NC_, SU], BF16))
            txs = Tok()
            aT = st.enter_context(_sbt("aT", [128, 8, SU], BF16))
            ta = Tok()
            rows = Ring(st, "rows", 2, [128, D], F32)
            wg = Ring(st, "wg", 4, [128, NC_, 256], BF16)
            wd = Ring(st, "wd", 3, [128, 8, 512], BF16)
            bdr = Ring(st, "bdr", 2, [1, D], BF16)
            yr = Ring(st, "yr", 3, [128, 512], F32)
            gg = Ring(st, "gg", 2, [128, 512], F32)
            sg = Ring(st, "sg", 2, [128, 512], F32)
            ll = Ring(st, "ll", 2, [128, 512], F32)
            ph = PRing([0, 1, 2, 3])
            py = PRing([4, 5])
            ptr = PRing([6, 7])
            for e in range(E):
                wsrc = ext[f"wgu_{li}_{e}"]
                wdsrc = ext[f"wdn_{li}_{e}"].rearrange("(c p) n -> p c n", p=128)
                for u in range(CAP // SU):
                    r0 = (e % EPP) * CAP + u * SU
                    XS, YS = XSp[e // EPP], YSp[e // EPP]
                    bd, tbd = bdr.next()
                    k.dma(pool, lambda: nc.gpsimd.dma_start(out=bd[:], in_=ext[f"moe_b_down{li}"][e:e + 1, :]), w=[tbd])
                    for s_ in range(SU // 128):
                        rw, trw = rows.next()
                        LD(sp, rw[:], XS[r0 + s_ * 128: r0 + (s_ + 1) * 128, :], w=[trw])
                        for g in range(4):
                            ps, tp = ptr.next()
                            for j4 in range(4):
                                c = g * 4 + j4
                                TR(ps[:, j4 * 128:(j4 + 1) * 128], rw[:, c * 128:(c + 1) * 128], r=[trw, tconst], w=[tp])
                            CP(act if g % 2 else dve, xsT[:, g * 4:(g + 1) * 4, s_ * 128:(s_ + 1) * 128],
                               ps[:].rearrange("p (j t) -> p j t", j=4), r=[tp], w=[txs])
                    for fc in range(8):
                        w_, twg = wg.next()
                        k.dma(pool, lambda: nc.gpsimd.dma_start(out=w_[:], in_=wsrc[fc]), w=[twg])
                        for (o, n) in NTS:
                            pg_, tpg = ph.next()
                            pl_, tpl = ph.next()
                            for c in range(NC_):
                                MM(pg_[:, 0:n], w_[:, c, 0:128], xsT[:, c, o:o + n], c == 0, c == NC_ - 1, r=[twg, txs], w=[tpg], inc=(c == NC_ - 1))
                            for c in range(NC_):
                                MM(pl_[:, 0:n], w_[:, c, 128:256], xsT[:, c, o:o + n], c == 0, c == NC_ - 1, r=[twg, txs], w=[tpl], inc=(c == NC_ - 1))
                            g_, tg = gg.next()
                            s_t, ts = sg.next()
                            l_, tl = ll.next()
                            TS(dve, g_[:, 0:n], pg_[:, 0:n], bgu[:, e, fc:fc + 1], 7.0, ALU.add, ALU.min, r=[tpg, tw], w=[tg])
                            ACT(s_t[:, 0:n], g_[:, 0:n], AF.Sigmoid, r=[tg], w=[ts], scale=1.702)
                            TS(dve, l_[:, 0:n], pl_[:, 0:n], bgu[:, e, 8 + fc:9 + fc], 7.0, ALU.add, ALU.min, r=[tpl, tw], w=[tl])
                            TS(pool, l_[:, 0:n], l_[:, 0:n], -7.0, 1.0, ALU.max, ALU.add, r=[tl], w=[tl])
                            TT(pool, g_[:, 0:n], g_[:, 0:n], s_t[:, 0:n], ALU.mult, r=[tg, ts], w=[tg])
                            TT(dve, aT[:, fc, o:o + n], g_[:, 0:n], l_[:, 0:n], ALU.mult, r=[tg, tl], w=[ta])
                    for n4 in range(4):
                        w_, twd = wd.next()
                        k.dma(pool, lambda: nc.gpsimd.dma_start(out=w_[:], in_=wdsrc[:, :, n4 * 512:(n4 + 1) * 512]), w=[twd])
                        for s_ in range(SU // 128):
                            ps, tp = py.next()
                            for fc in range(8):
                                MM(ps[:], aT[:, fc, s_ * 128:(s_ + 1) * 128], w_[:, fc, :], fc == 0, False, r=[ta, twd], w=[tp], inc=False)
                            MM(ps[:], onesb[0:1, :], bd[0:1, n4 * 512:(n4 + 1) * 512], False, True, r=[tconst, tbd, ta, twd], w=[tp])
                            y_, ty = yr.next()
                            CP(act if s_ % 2 else dve, y_[:], ps[:], r=[tp], w=[ty])
                            ST(sp, YS[r0 + s_ * 128: r0 + (s_ + 1) * 128, n4 * 512:(n4 + 1) * 512], y_[:], r=[ty])
            k.barrier()
        with ExitStack() as st:
            gt = st.enter_context(_sbt("lg2", [128, D], F32))
            bt = st.enter_context(_sbt("lb2", [128, D], F32))
            tw = Tok()
            LD(act, gt[:], ext[f"ln2_g{li}"].partition_broadcast(128), w=[tw])
            LD(act, bt[:], ext[f"ln2_b{li}"].partition_broadcast(128), w=[tw])
            x1r = Ring(st, "x1m", 2, [128, D], F32)
            ykr = Ring(st, "ykr", 4, [128, D], F32)
            accr = Ring(st, "accr", 2, [128, D], F32)
            xtr = Ring(st, "xtm", 2, [128, NC_, 128], BF16)
            g4r = Ring(st, "g4m", 2, [128, TOPK], F32)
            dir_ = Ring(st, "dim", 2, [128, G, TOPK], I32)
            junk = st.enter_context(_sbt("junk2", [128, D], BF16))
            stat = st.enter_context(_sbt("stat2", [128, 8], F32))
            tj, tstat = Tok(), Tok()
            ptr = PRing([0, 1, 2, 3])
            for ti in range(NT):
                tsl = slice(ti * 128, (ti + 1) * 128)
                x1, tx1 = x1r.next()
                LD(sp, x1[:], X1[tsl, :], w=[tx1])
                g4, tg4 = g4r.next()
                LD(sp, g4[:], GD[tsl, :], w=[tg4])
                di, tdi = dir_.next()
                LD(sp, di[:].rearrange("p g k -> p (g k)"), DI[tsl, :], w=[tdi])
                acc, tacc = accr.next()
                TS(dve, acc[:], x1[:], alpha, None, ALU.mult, None, r=[tx1], w=[tacc])
                for kk_ in range(TOPK):
                    yk, tyk = ykr.next()
                    for g in range(G):
                        k.dma(pool, lambda: nc.gpsimd.indirect_dma_start(out=yk[:, :], out_offset=None, in_=YSp[g][:, :],
                                                                        in_offset=bass.IndirectOffsetOnAxis(ap=di[:, g, kk_:kk_ + 1], axis=0),
                                                                        bounds_check=bnd, oob_is_err=False), r=[tdi], w=[tyk])
                    STT(dve, acc[:], yk[:], g4[:, kk_:kk_ + 1], acc[:], ALU.mult, ALU.add, r=[tyk, tg4, tacc], w=[tacc])
                layer_norm((junk, tj, stat, tstat), acc, tacc, gt, bt, tw, x1, tx1)
                ST(sp, Xout[tsl, :], x1[:], r=[tx1])
                if not last:
                    xt_, txt = xtr.next()
                    transpose_tile(ptr, x1, tx1, xt_, txt)
                    ST(sp, XoutT.rearrange("(c p) t -> p c t", p=128)[:, :, tsl], xt_[:], r=[txt])
            k.barrier()

    kstop = int(os.environ.get("KSTOP", "999"))
    stages = []
    for li in range(L):
        j = li // 2
        Xin = ext["x"] if li == 0 else XA
        last = li == L - 1
        if li % 2 == 0:
            stages.append(lambda j=j: mla_proj(j, XT))
            stages.append(lambda: attention(True))
            wo_ap = ext[f"mla_w_o{j}"]
        else:
            stages.append(lambda j=j: moba_proj(j, XT))
            stages.append(lambda: attention(False))
            wo_ap = ext[f"moba_w_o{j}"]
        stages.append(lambda li=li, wo_ap=wo_ap, Xin=Xin: post_attn(li, wo_ap, Xin))
        stages.append(lambda li=li, last=last: moe(li, XA, XT, last))
    for si, fn in enumerate(stages):
        if si >= kstop:
            break
        fn()
    k.barrier()
    for c in range(0, T, 1024):
        k.dma(sp, lambda: nc.sync.dma_start(out=out[c:c + 1024, :], in_=XA[c:c + 1024, :]))
    k.barrier(only=[sp])
    es.close()
    return nc


def _consts(B, S):
    c = {}
    c["identf"] = np.eye(128, dtype=np.float32)
    c["identb"] = _bf(np.eye(128, dtype=np.float32))
    c["onesb"] = _bf(np.ones((128, 128), np.float32))
    kk = np.arange(128)[:, None, None]
    jj = np.arange(4)[None, :, None]
    qq = np.arange(512)[None, None, :]
    c["cm"] = _bf(np.where(qq >= 128 * jj + kk, 0.0, NEG).astype(np.float32))
    inv = (10000.0 ** (-np.arange(0, 64, 2, dtype=np.float32) / 64)).astype(np.float32)
    c["invf"] = np.concatenate([inv, inv]).reshape(64, 1).astype(np.float32)
    c16 = np.zeros((128, 2, 16), np.float32)
    c16[:, 1, :] = -1e30
    c["c16"] = c16
    c["ustr"] = _bf(np.triu(np.ones((128, 128), np.float32), 1))
    slopes = (2.0 ** (-8.0 * np.arange(1, H + 1, dtype=np.float32) / H)).astype(np.float32)
    s1 = slopes.astype(ml_dtypes.bfloat16).astype(np.float32)
    s2 = (slopes - s1).astype(ml_dtypes.bfloat16).astype(np.float32)
    s3 = (slopes - s1 - s2).astype(ml_dtypes.bfloat16).astype(np.float32)
    sl = np.stack([s1, s2, s3, s1, s2, s3], 1)
    lk = np.zeros((B, H, 28, S), np.float32)
    rq = np.zeros((B, H, 28, S), np.float32)
    blk = np.arange(S) // 256
    lk[:, :, :16, :] = (np.arange(16)[:, None] == blk[None, :]).astype(np.float32)[None, None]
    lk[:, :, 22:28, :] = sl[None, :, :, None]
    rq[:, :, 16:22, :] = sl[None, :, :, None]
    c["lkc"] = _bf(lk)
    c["rqc"] = _bf(rq)
    return c


def cap_of(T, E):
    return (((T * TOPK // E) * 5 // 4) + 255) // 256 * 256


def prepare(inputs, B, S, L, E):
    m = dict(_consts(B, S))
    m["ecap"] = np.ascontiguousarray(np.broadcast_to((np.arange(E) * cap_of(B * S, E)).astype(np.float32)[None, :], (128, E)))
    x = np.asarray(inputs["x"], np.float32).reshape(B * S, D)
    m["x"] = np.ascontiguousarray(x)
    m["xT"] = np.ascontiguousarray(x.T)
    m["pos"] = np.ascontiguousarray(np.asarray(inputs["positions"]).astype(np.int32).reshape(B, S))
    for j in range((L + 1) // 2):
        w_in = np.asarray(inputs["mla_w_in"][j])
        m[f"mla_w_in{j}"] = np.ascontiguousarray(w_in)
        m[f"mla_w_in_sw{j}"] = np.ascontiguousarray(np.concatenate([w_in[:, 1056:1088], w_in[:, 1024:1056]], 1))
        m[f"mla_g_q{j}"] = np.ascontiguousarray(np.asarray(inputs["mla_g_q"][j]).reshape(4, 128).T)
        m[f"mla_g_kv{j}"] = np.ascontiguousarray(np.asarray(inputs["mla_g_kv"][j]).reshape(4, 128).T)
        wq = np.asarray(inputs["mla_w_qb"][j])
        m[f"mla_w_qb{j}"] = np.ascontiguousarray(wq)
        w3 = wq.reshape(512, H, 192)
        m[f"mla_w_qb_sw{j}"] = np.ascontiguousarray(np.concatenate([w3[:, :, 160:192], w3[:, :, 128:160]], 2).reshape(512, H * 64))
        m[f"mla_w_kvb{j}"] = np.ascontiguousarray(np.asarray(inputs["mla_w_kvb"][j]))
        m[f"mla_w_o{j}"] = np.ascontiguousarray(np.asarray(inputs["mla_w_o"][j]))
    for j in range(L // 2):
        m[f"moba_w_qkv{j}"] = np.ascontiguousarray(np.asarray(inputs["moba_w_qkv"][j]))
        m[f"moba_w_o{j}"] = np.ascontiguousarray(np.asarray(inputs["moba_w_o"][j]))
    for li in range(L):
        for nm in ("ln1_g", "ln1_b", "ln2_g", "ln2_b"):
            m[f"{nm}{li}"] = np.ascontiguousarray(np.asarray(inputs[nm][li]).reshape(1, D))
        m[f"moe_w_router{li}"] = np.ascontiguousarray(np.asarray(inputs["moe_w_router"][li]))
        m[f"moe_b_router{li}"] = np.ascontiguousarray(np.asarray(inputs["moe_b_router"][li]).reshape(1, E))
        m[f"moe_b_gu_t{li}"] = np.ascontiguousarray(np.asarray(inputs["moe_b_gu"][li]).reshape(E, 16, 128).transpose(2, 0, 1))
        m[f"moe_b_down{li}"] = np.ascontiguousarray(np.asarray(inputs["moe_b_down"][li]))
        for e in range(E):
            w5 = np.asarray(inputs["moe_w_gu"][li, e]).reshape(NC_, 128, 2, 8, 128)
            m[f"wgu_{li}_{e}"] = np.ascontiguousarray(w5.transpose(3, 1, 0, 2, 4)).reshape(8, 128, NC_, 256)
            m[f"wdn_{li}_{e}"] = np.ascontiguousarray(np.asarray(inputs["moe_w_down"][li, e]))
    return m


_NPDT = {np.dtype(np.float32): F32, np.dtype(np.int32): I32, np.dtype(ml_dtypes.bfloat16): BF16}


def run(inputs, B, S, L, E):
    m = prepare(inputs, B, S, L, E)
    names = {n: (a.shape, _NPDT[a.dtype]) for n, a in m.items()}
    nc = build(B, S, L, E, names)
    res = run_bass_kernel_spmd(nc, [m], core_ids=[0])
    return np.asarray(res.results[0]["out"], np.float32).reshape(B, S, D)


def run_multi(inputs, B, S, L, E):
    x = np.asarray(inputs["x"], np.float32).reshape(B, S, D)
    pos = np.asarray(inputs["positions"]).astype(np.int32).reshape(B, S)
    one = dict(inputs)
    one["x"] = x[0:1]
    one["positions"] = pos[0:1]
    base = prepare(one, 1, S, L, E)
    in_maps = []
    for b in range(B):
        m = dict(base)
        m["x"] = np.ascontiguousarray(x[b])
        m["xT"] = np.ascontiguousarray(x[b].T)
        m["pos"] = np.ascontiguousarray(pos[b:b + 1])
        in_maps.append(m)
    names = {n: (a.shape, _NPDT[a.dtype]) for n, a in base.items()}
    nc = build(1, S, L, E, names)
    res = run_bass_kernel_spmd(nc, in_maps, core_ids=list(range(B)))
    return np.stack([np.asarray(r["out"], np.float32).reshape(S, D) for r in res.results], 0)


def kernel(**inputs):
    return run_multi(inputs, 4, 4096, 4, 32)
```
